# Optimizing a Trainium2 kernel written in Bass

```python
import math
import jax
import jax.numpy as jnp
from jax import lax
import numpy as np

D_MODEL = 1024
BATCH = 32
SEQ = 2048
DEPTH = 1

MIX_WIDTH = D_MODEL
RG_WIDTH = MIX_WIDTH // 2
RG_BLOCKS = 8
RG_BLOCK = RG_WIDTH // RG_BLOCKS
CONV_W = 4
RG_C = 8.0
NSA_WIDTH = MIX_WIDTH - RG_WIDTH
NSA_HEADS = 8
HEAD_DIM = NSA_WIDTH // NSA_HEADS
NSA_KV = 2
NSA_HPG = NSA_HEADS // NSA_KV
KV_W = NSA_KV * HEAD_DIM
CMP_L = 32
CMP_STRIDE = 16
CMP_HIDDEN = 256
SEL_L = 64
N_SEL = 8
WINDOW = 512
Q_BLK = 64
NUM_BUCKETS = 32
MAX_DIST = 128
MEM_LEN = 256
X_HEADS = 4
X_HEAD_DIM = D_MODEL // X_HEADS
N_GROUPS = 4
EXP_PER_GROUP = 8
N_EXPERTS = N_GROUPS * EXP_PER_GROUP
TOP_K_IN_GROUP = 2
D_EXPERT = 512
MOE_BLK = 128
EPS = 1e-6
NEG_INF = -1e30
SEL_FORCE = 1e9
PROJ_SIZES = [RG_WIDTH, RG_WIDTH, NSA_WIDTH, KV_W, KV_W, KV_W, KV_W, KV_W, KV_W, 3 * NSA_HEADS]
PROJ_COLS = sum(PROJ_SIZES)

kernel_name = 'hymba_rglru_nsa_hiermoe_layer'


def rmsnorm(x, g):
    x32 = x.astype(jnp.float32)
    y = x32 * lax.rsqrt(jnp.mean(x32 * x32, axis=-1, keepdims=True) + EPS)
    return (y * g.astype(jnp.float32)).astype(x.dtype)


def masked_softmax(logits, mask):
    return jax.nn.softmax(jnp.where(mask, logits, NEG_INF), axis=-1)


def rel_bucket(dist):
    n = jnp.maximum(dist, 0)
    max_exact = NUM_BUCKETS // 2
    nf = jnp.maximum(n, 1).astype(jnp.float32)
    large = max_exact + (jnp.log(nf / max_exact) / math.log(MAX_DIST / max_exact)
                         * (NUM_BUCKETS - max_exact)).astype(jnp.int32)
    large = jnp.minimum(large, NUM_BUCKETS - 1)
    return jnp.where(n < max_exact, n, large)


def _lru_combine(c1, c2):
    a1, b1 = c1
    a2, b2 = c2
    return a1 * a2, a2 * b1 + b2


def rglru_group(u, gate, conv_w, conv_b, w_r, b_r, w_i, b_i, lam):
    bsz, seq, width = u.shape
    uc = lax.conv_general_dilated(u, conv_w, window_strides=(1,), padding=[(CONV_W - 1, 0)],
                                  dimension_numbers=('NWC', 'WIO', 'NWC'),
                                  feature_group_count=width) + conv_b
    ub = uc.reshape(bsz, seq, RG_BLOCKS, RG_BLOCK)
    r = jax.nn.sigmoid((jnp.einsum('bshi,hij->bshj', ub, w_r).reshape(bsz, seq, width) + b_r).astype(jnp.float32))
    i_g = jax.nn.sigmoid((jnp.einsum('bshi,hij->bshj', ub, w_i).reshape(bsz, seq, width) + b_i).astype(jnp.float32))
    log_a = -RG_C * r * jax.nn.softplus(-lam.astype(jnp.float32))
    a = jnp.exp(log_a)
    b_in = jnp.sqrt(-jnp.expm1(2.0 * log_a)) * i_g * uc.astype(jnp.float32)
    _, h = lax.associative_scan(_lru_combine, (a, b_in), axis=1)
    return (jax.nn.gelu(gate.astype(jnp.float32)) * h).astype(u.dtype)


def nsa_group(q, kc, vc, ks, vs, kw, vw, gate_logits, rel_bias, g_q, g_kc, g_ks, g_kw,
              pos_k, pos_v, ck_w1, ck_w2, cv_w1, cv_w2):
    bsz, seq = q.shape[:2]
    dt = q.dtype
    q = rmsnorm(q, g_q)
    ks = rmsnorm(ks, g_ks)
    kw = rmsnorm(kw, g_kw)
    scale = HEAD_DIM ** -0.5

    n_cmp = (seq - CMP_L) // CMP_STRIDE + 1
    cmp_start = jnp.arange(n_cmp, dtype=jnp.int32) * CMP_STRIDE
    cmp_end = cmp_start + CMP_L - 1
    cmp_center = cmp_start + CMP_L // 2
    gidx = cmp_start[:, None] + jnp.arange(CMP_L, dtype=jnp.int32)[None, :]

    def compress(t, pos, w1, w2):
        blk = t[:, gidx] + pos[None, None, :, None, :]
        blk = blk.transpose(0, 1, 3, 2, 4).reshape(bsz, n_cmp, NSA_KV, CMP_L * HEAD_DIM)
        return jax.nn.gelu(blk @ w1) @ w2

    k_cmp = rmsnorm(compress(kc, pos_k, ck_w1, ck_w2), g_kc)
    v_cmp = compress(vc, pos_v, cv_w1, cv_w2)

    n_sel = seq // SEL_L
    k_sel = min(N_SEL, n_sel)
    sel_start = jnp.arange(n_sel, dtype=jnp.int32) * SEL_L
    overlap = ((cmp_start[:, None] <= sel_start[None, :] + SEL_L - 1) &
               (cmp_end[:, None] >= sel_start[None, :])).astype(jnp.float32)
    ks_blk = ks.reshape(bsz, n_sel, SEL_L, NSA_KV, HEAD_DIM).transpose(0, 3, 1, 2, 4)
    vs_blk = vs.reshape(bsz, n_sel, SEL_L, NSA_KV, HEAD_DIM).transpose(0, 3, 1, 2, 4)
    gather = jax.vmap(jax.vmap(lambda kb, ib: kb[ib]))

    kw_pad = jnp.pad(kw, ((0, 0), (WINDOW, 0), (0, 0), (0, 0)))
    vw_pad = jnp.pad(vw, ((0, 0), (WINDOW, 0), (0, 0), (0, 0)))

    table_g = rel_bias.reshape(NUM_BUCKETS, NSA_KV, NSA_HPG).transpose(1, 0, 2)
    g_arange = jnp.arange(NSA_KV)[None, :, None, None]
    gates = jax.nn.sigmoid(gate_logits.astype(jnp.float32))
    n_blk = seq // Q_BLK
    q_b = q.reshape(bsz, n_blk, Q_BLK, NSA_KV, NSA_HPG, HEAD_DIM).transpose(1, 0, 2, 3, 4, 5)
    g_b = gates.reshape(bsz, n_blk, Q_BLK, NSA_KV, NSA_HPG, 3).transpose(1, 0, 2, 3, 4, 5)

    def block_fn(args):
        i, qb, gb = args
        t = i * Q_BLK + jnp.arange(Q_BLK, dtype=jnp.int32)
        lc = jnp.einsum('bqgpd,bcgd->bgpqc', qb, k_cmp).astype(jnp.float32) * scale
        lc = lc + table_g[:, rel_bucket(t[:, None] - cmp_center[None, :])].transpose(0, 3, 1, 2)[None]
        mc = cmp_end[None, :] <= t[:, None]
        pc = jnp.where(mc, masked_softmax(lc, mc), 0.0)
        o_c = jnp.einsum('bgpqc,bcgd->bqgpd', pc.astype(dt), v_cmp)
        imp = jnp.einsum('bgpqc,cn->bgqn', pc, overlap)
        cur = t // SEL_L
        blk_ids = jnp.arange(n_sel, dtype=jnp.int32)[None, :]
        forced = (blk_ids == 0) | (blk_ids == cur[:, None]) | (blk_ids == cur[:, None] - 1)
        future = blk_ids > cur[:, None]
        imp = jnp.where(forced, SEL_FORCE, jnp.where(future, -SEL_FORCE, imp))
        _, sel = lax.top_k(imp, k_sel)
        k_s = gather(ks_blk, sel).reshape(bsz, NSA_KV, Q_BLK, k_sel * SEL_L, HEAD_DIM)
        v_s = gather(vs_blk, sel).reshape(bsz, NSA_KV, Q_BLK, k_sel * SEL_L, HEAD_DIM)
        kpos = (sel[..., None] * SEL_L + jnp.arange(SEL_L, dtype=jnp.int32)).reshape(bsz, NSA_KV, Q_BLK, k_sel * SEL_L)
        dist = t[None, None, :, None] - kpos
        bs = table_g[g_arange, rel_bucket(dist)].transpose(0, 1, 4, 2, 3)
        ls = jnp.einsum('bqgpd,bgqkd->bgpqk', qb, k_s).astype(jnp.float32) * scale + bs
        ps = masked_softmax(ls, (dist >= 0)[:, :, None])
        o_s = jnp.einsum('bgpqk,bgqkd->bqgpd', ps.astype(dt), v_s)
        k_w = lax.dynamic_slice_in_dim(kw_pad, i * Q_BLK, WINDOW + Q_BLK, axis=1)
        v_w = lax.dynamic_slice_in_dim(vw_pad, i * Q_BLK, WINDOW + Q_BLK, axis=1)
        wpos = i * Q_BLK - WINDOW + jnp.arange(WINDOW + Q_BLK, dtype=jnp.int32)
        dw = t[:, None] - wpos[None, :]
        mw = (dw >= 0) & (dw < WINDOW) & (wpos[None, :] >= 0)
        lw = jnp.einsum('bqgpd,bkgd->bgpqk', qb, k_w).astype(jnp.float32) * scale
        lw = lw + table_g[:, rel_bucket(dw)].transpose(0, 3, 1, 2)[None]
        pw = masked_softmax(lw, mw)
        o_w = jnp.einsum('bgpqk,bkgd->bqgpd', pw.astype(dt), v_w)
        out = gb[..., 0:1] * o_c + gb[..., 1:2] * o_s + gb[..., 2:3] * o_w
        return out.astype(dt)

    o = lax.map(block_fn, (jnp.arange(n_blk, dtype=jnp.int32), q_b, g_b))
    return o.transpose(1, 0, 2, 3, 4, 5).reshape(bsz, seq, NSA_WIDTH)


def memory_cross_attention(h, mem, g_h, g_m, w_q, w_kv, w_o, g_q, g_k):
    bsz, seq, _ = h.shape
    hn = rmsnorm(h, g_h)
    mn = rmsnorm(mem, g_m)
    q = rmsnorm((hn @ w_q).reshape(bsz, seq, X_HEADS, X_HEAD_DIM), g_q)
    k, v = jnp.split(mn @ w_kv, 2, axis=-1)
    k = rmsnorm(k.reshape(bsz, -1, X_HEADS, X_HEAD_DIM), g_k)
    v = v.reshape(bsz, -1, X_HEADS, X_HEAD_DIM)
    logits = jnp.einsum('bshd,bmhd->bhsm', q, k).astype(jnp.float32) * (X_HEAD_DIM ** -0.5)
    p = jax.nn.softmax(logits, axis=-1)
    o = jnp.einsum('bhsm,bmhd->bshd', p.astype(v.dtype), v).reshape(bsz, seq, D_MODEL)
    return o @ w_o


def hierarchical_moe(h, g_norm, w_grp, b_grp, w_exp, b_exp, w1, w3, w2):
    xt = rmsnorm(h, g_norm).reshape(-1, D_MODEL)
    n_tok = xt.shape[0]
    gl = (xt @ w_grp).astype(jnp.float32) + b_grp
    gp = jax.nn.softmax(gl, axis=-1)
    g_sel = jnp.argmax(gl, axis=-1)
    p_g = jnp.take_along_axis(gp, g_sel[:, None], axis=1)[:, 0]
    el = (xt @ w_exp).astype(jnp.float32).reshape(n_tok, N_GROUPS, EXP_PER_GROUP) + b_exp.reshape(N_GROUPS, EXP_PER_GROUP)
    el = jnp.take_along_axis(el, g_sel[:, None, None], axis=1)[:, 0]
    top_p, top_e = lax.top_k(jax.nn.softmax(el, axis=-1), TOP_K_IN_GROUP)
    top_p = top_p / jnp.sum(top_p, axis=-1, keepdims=True)
    weights = (p_g[:, None] * top_p).reshape(-1)
    expert = (g_sel[:, None] * EXP_PER_GROUP + top_e).reshape(-1).astype(jnp.int32)
    token = jnp.repeat(jnp.arange(n_tok, dtype=jnp.int32), TOP_K_IN_GROUP)
    n_slots = n_tok * TOP_K_IN_GROUP
    order = jnp.argsort(expert)
    s_exp, s_tok, s_w = expert[order], token[order], weights[order]
    counts = jnp.bincount(expert, length=N_EXPERTS)
    starts = jnp.cumsum(counts) - counts
    pcounts = (counts + MOE_BLK - 1) // MOE_BLK * MOE_BLK
    pends = jnp.cumsum(pcounts)
    pstarts = pends - pcounts
    dest = pstarts[s_exp] + (jnp.arange(n_slots, dtype=jnp.int32) - starts[s_exp])
    n_blocks = -(-n_slots // MOE_BLK) + N_EXPERTS
    n_pad = n_blocks * MOE_BLK
    buf_tok = jnp.zeros((n_pad,), jnp.int32).at[dest].set(s_tok)
    buf_w = jnp.zeros((n_pad,), jnp.float32).at[dest].set(s_w)
    blk_exp = jnp.minimum(jnp.searchsorted(pends, jnp.arange(n_blocks, dtype=jnp.int32) * MOE_BLK, side='right'),
                          N_EXPERTS - 1)

    def expert_block(args):
        e, tok = args
        xb = xt[tok]
        return (jax.nn.silu(xb @ w1[e]) * (xb @ w3[e])) @ w2[e]

    yb = lax.map(expert_block, (blk_exp, buf_tok.reshape(n_blocks, MOE_BLK))).reshape(n_pad, D_MODEL)
    y = jnp.zeros((n_tok, D_MODEL), jnp.float32).at[buf_tok].add(yb.astype(jnp.float32) * buf_w[:, None])
    return y.reshape(h.shape).astype(h.dtype)


def setup_inputs(seed: int = 0) -> dict:
    key = jax.random.key(seed)
    kit = iter(jax.random.split(key, 64))
    f32 = jnp.float32
    L = DEPTH

    def nrm(shape, scale):
        return jax.random.normal(next(kit), shape, f32) * scale

    def gain(shape):
        return 1.0 + 0.02 * jax.random.normal(next(kit), shape, f32)

    x = nrm((BATCH, SEQ, D_MODEL), 1.0)
    mem = nrm((BATCH, MEM_LEN, D_MODEL), 1.0)
    rel_bias = nrm((NUM_BUCKETS, NSA_HEADS), 0.5)
    norm_mix = gain((L, D_MODEL))
    w_in = nrm((L, D_MODEL, PROJ_COLS), D_MODEL ** -0.5)
    rg_conv_w = nrm((L, CONV_W, 1, RG_WIDTH), CONV_W ** -0.5)
    rg_conv_b = nrm((L, RG_WIDTH), 0.02)
    rg_w_r = nrm((L, RG_BLOCKS, RG_BLOCK, RG_BLOCK), RG_BLOCK ** -0.5)
    rg_b_r = nrm((L, RG_WIDTH), 0.02)
    rg_w_i = nrm((L, RG_BLOCKS, RG_BLOCK, RG_BLOCK), RG_BLOCK ** -0.5)
    rg_b_i = nrm((L, RG_WIDTH), 0.02)
    a0 = jax.random.uniform(next(kit), (L, RG_WIDTH), f32, minval=0.9, maxval=0.999) ** (1.0 / RG_C)
    rg_lambda = jnp.log(a0) - jnp.log1p(-a0)
    nsa_g_q = gain((L, HEAD_DIM))
    nsa_g_kc = gain((L, HEAD_DIM))
    nsa_g_ks = gain((L, HEAD_DIM))
    nsa_g_kw = gain((L, HEAD_DIM))
    cmp_pos_k = nrm((L, CMP_L, HEAD_DIM), 0.02)
    cmp_pos_v = nrm((L, CMP_L, HEAD_DIM), 0.02)
    cmp_k_w1 = nrm((L, CMP_L * HEAD_DIM, CMP_HIDDEN), (CMP_L * HEAD_DIM) ** -0.5)
    cmp_k_w2 = nrm((L, CMP_HIDDEN, HEAD_DIM), CMP_HIDDEN ** -0.5)
    cmp_v_w1 = nrm((L, CMP_L * HEAD_DIM, CMP_HIDDEN), (CMP_L * HEAD_DIM) ** -0.5)
    cmp_v_w2 = nrm((L, CMP_HIDDEN, HEAD_DIM), CMP_HIDDEN ** -0.5)
    out_g_rg = gain((L, RG_WIDTH))
    out_g_nsa = gain((L, NSA_WIDTH))
    w_out = nrm((L, MIX_WIDTH, D_MODEL), MIX_WIDTH ** -0.5)
    norm_x = gain((L, D_MODEL))
    norm_mem = gain((L, D_MODEL))
    xa_w_q = nrm((L, D_MODEL, D_MODEL), D_MODEL ** -0.5)
    xa_w_kv = nrm((L, D_MODEL, 2 * D_MODEL), D_MODEL ** -0.5)
    xa_w_o = nrm((L, D_MODEL, D_MODEL), D_MODEL ** -0.5)
    xa_g_q = gain((L, X_HEAD_DIM))
    xa_g_k = gain((L, X_HEAD_DIM))
    norm_moe = gain((L, D_MODEL))
    router_g_w = nrm((L, D_MODEL, N_GROUPS), D_MODEL ** -0.5)
    router_g_b = nrm((L, N_GROUPS), 0.01)
    router_e_w = nrm((L, D_MODEL, N_EXPERTS), D_MODEL ** -0.5)
    router_e_b = nrm((L, N_EXPERTS), 0.01)
    exp_w1 = nrm((L, N_EXPERTS, D_MODEL, D_EXPERT), D_MODEL ** -0.5)
    exp_w3 = nrm((L, N_EXPERTS, D_MODEL, D_EXPERT), D_MODEL ** -0.5)
    exp_w2 = nrm((L, N_EXPERTS, D_EXPERT, D_MODEL), D_EXPERT ** -0.5)
    return {'x': x, 'mem': mem, 'rel_bias': rel_bias, 'norm_mix': norm_mix, 'w_in': w_in,
            'rg_conv_w': rg_conv_w, 'rg_conv_b': rg_conv_b, 'rg_w_r': rg_w_r, 'rg_b_r': rg_b_r,
            'rg_w_i': rg_w_i, 'rg_b_i': rg_b_i, 'rg_lambda': rg_lambda,
            'nsa_g_q': nsa_g_q, 'nsa_g_kc': nsa_g_kc, 'nsa_g_ks': nsa_g_ks, 'nsa_g_kw': nsa_g_kw,
            'cmp_pos_k': cmp_pos_k, 'cmp_pos_v': cmp_pos_v, 'cmp_k_w1': cmp_k_w1, 'cmp_k_w2': cmp_k_w2,
            'cmp_v_w1': cmp_v_w1, 'cmp_v_w2': cmp_v_w2, 'out_g_rg': out_g_rg, 'out_g_nsa': out_g_nsa,
            'w_out': w_out, 'norm_x': norm_x, 'norm_mem': norm_mem, 'xa_w_q': xa_w_q, 'xa_w_kv': xa_w_kv,
            'xa_w_o': xa_w_o, 'xa_g_q': xa_g_q, 'xa_g_k': xa_g_k, 'norm_moe': norm_moe,
            'router_g_w': router_g_w, 'router_g_b': router_g_b, 'router_e_w': router_e_w,
            'router_e_b': router_e_b, 'exp_w1': exp_w1, 'exp_w3': exp_w3, 'exp_w2': exp_w2}


def reference(x, mem, rel_bias, norm_mix, w_in, rg_conv_w, rg_conv_b, rg_w_r, rg_b_r, rg_w_i, rg_b_i,
              rg_lambda, nsa_g_q, nsa_g_kc, nsa_g_ks, nsa_g_kw, cmp_pos_k, cmp_pos_v, cmp_k_w1, cmp_k_w2,
              cmp_v_w1, cmp_v_w2, out_g_rg, out_g_nsa, w_out, norm_x, norm_mem, xa_w_q, xa_w_kv, xa_w_o,
              xa_g_q, xa_g_k, norm_moe, router_g_w, router_g_b, router_e_w, router_e_b,
              exp_w1, exp_w3, exp_w2):
    bsz, seq, _ = x.shape
    offsets = np.cumsum(PROJ_SIZES)[:-1].tolist()
    h = x
    for l in range(DEPTH):
        proj = rmsnorm(h, norm_mix[l]) @ w_in[l]
        u, gate, q, kc, vc, ks, vs, kw, vw, gl = jnp.split(proj, offsets, axis=-1)
        y_rg = rglru_group(u, gate, rg_conv_w[l], rg_conv_b[l], rg_w_r[l], rg_b_r[l],
                           rg_w_i[l], rg_b_i[l], rg_lambda[l])
        kvs = lambda t: t.reshape(bsz, seq, NSA_KV, HEAD_DIM)
        y_nsa = nsa_group(q.reshape(bsz, seq, NSA_HEADS, HEAD_DIM), kvs(kc), kvs(vc), kvs(ks), kvs(vs),
                          kvs(kw), kvs(vw), gl.reshape(bsz, seq, NSA_HEADS, 3), rel_bias,
                          nsa_g_q[l], nsa_g_kc[l], nsa_g_ks[l], nsa_g_kw[l], cmp_pos_k[l], cmp_pos_v[l],
                          cmp_k_w1[l], cmp_k_w2[l], cmp_v_w1[l], cmp_v_w2[l])
        mixed = jnp.concatenate([rmsnorm(y_rg, out_g_rg[l]), rmsnorm(y_nsa, out_g_nsa[l])], axis=-1)
        h = h + mixed @ w_out[l]
        h = h + memory_cross_attention(h, mem, norm_x[l], norm_mem[l], xa_w_q[l], xa_w_kv[l], xa_w_o[l],
                                       xa_g_q[l], xa_g_k[l])
        h = h + hierarchical_moe(h, norm_moe[l], router_g_w[l], router_g_b[l], router_e_w[l], router_e_b[l],
                                 exp_w1[l], exp_w3[l], exp_w2[l])
    return h
```

```python
import math
from contextlib import ExitStack
import numpy as np
import concourse.bass as bass
import concourse.mybir as mybir
from concourse.bass_utils import run_bass_kernel_spmd

F32 = mybir.dt.float32
BF16 = mybir.dt.bfloat16
I32 = mybir.dt.int32
AF = mybir.ActivationFunctionType
ALU = mybir.AluOpType
AX = mybir.AxisListType

T = 2048
D = 1024
NT = 16
PC = 2328
ML = 256
NEXP = 32
BS = 256
BS_SH = 8
NEG = -30000.0
LW, LS, LC = 768, 512, 4096
KF = LW + LS + LC
N_CORES = 8


class Buf:
    __slots__ = ("w", "r", "name", "x")

    def __init__(self, name="", x=False):
        self.w = None
        self.r = {}
        self.name = name
        self.x = x


class TB:
    def __init__(self, t, name=""):
        self.t = t
        self.b = Buf(name)


def _bufs(xs):
    return [x.b if isinstance(x, TB) else x for x in xs]


class Sched:
    def __init__(self, nc, es, n_dma_sems=48):
        self.nc = nc
        self.eng = {"pe": nc.tensor, "act": nc.scalar, "dve": nc.vector, "pool": nc.gpsimd, "sp": nc.sync}
        self.sem = {}
        self.cnt = {}
        self.seen = {e: {} for e in self.eng}
        for e in self.eng:
            self.sem[e] = es.enter_context(nc.semaphore("s_" + e))
            self.cnt[e] = 0
        self.dsem = [es.enter_context(nc.semaphore("d%d" % i)) for i in range(n_dma_sems)]
        self.dcnt = [0] * n_dma_sems
        self.dq = [0, 0]

    def _semobj(self, key):
        return self.sem[key] if isinstance(key, str) else self.dsem[key]

    def _wait(self, e, ev):
        if ev is None:
            return
        key, val = ev
        if self.seen[e].get(key, 0) >= val:
            return
        self.eng[e].wait_ge(self._semobj(key), val)
        self.seen[e][key] = val

    def _deps(self, e, reads, writes, xreads=()):
        for b in list(reads) + list(xreads):
            if b.w is not None and not (e == "pe" and b.w[0] == "pe"):
                self._wait(e, b.w)
        for b in list(writes) + list(xreads):
            if b.w is not None and not (e == "pe" and b.w[0] == "pe"):
                self._wait(e, b.w)
            for ev in list(b.r.items()):
                if not (e == "pe" and ev[0] == "pe"):
                    self._wait(e, ev)

    def _mark(self, ev, reads, writes):
        key, val = ev
        for b in reads:
            b.r[key] = max(b.r.get(key, 0), val)
        for b in writes:
            b.w = ev
            b.r = {}

    def op(self, e, fn, reads=(), writes=(), force=()):
        reads = _bufs(reads)
        writes = _bufs(writes)
        xreads = [b for b in reads if b.x]
        reads = [b for b in reads if not b.x]
        self._deps(e, reads, writes, xreads)
        for ev_ in force:
            self._wait(e, ev_)
        ins = fn(self.eng[e])
        self.cnt[e] += 1
        ins.then_inc(self.sem[e], 1)
        ev = (e, self.cnt[e])
        self._mark(ev, reads, writes + xreads)
        return ev

    def _dma_common(self, e, reads, writes, issue):
        reads = _bufs(reads)
        writes = _bufs(writes)
        self._deps(e, reads, writes)
        half = len(self.dsem) // 2
        q = 0 if e == "sp" else 1
        i = q * half + self.dq[q]
        self.dq[q] = (self.dq[q] + 1) % half
        if self.dcnt[i] > 0:
            self._wait(e, (i, self.dcnt[i]))
        self.dcnt[i] += 16
        issue().then_inc(self.dsem[i], 16)
        ev = (i, self.dcnt[i])
        self._mark(ev, reads, writes)
        return ev

    def dma(self, e, out, in_, reads=(), writes=(), **kw):
        return self._dma_common(e, reads, writes, lambda: self.eng[e].dma_start(out=out, in_=in_, **kw))

    def gather(self, out, src, idx_ap, reads=(), writes=()):
        return self._dma_common("pool", reads, writes, lambda: self.nc.gpsimd.indirect_dma_start(
            out=out, out_offset=None, in_=src, in_offset=bass.IndirectOffsetOnAxis(ap=idx_ap, axis=0)))

    def scatter(self, dst, src, idx_ap, reads=(), writes=(), add=False):
        kw = {"compute_op": ALU.add} if add else {}
        return self._dma_common("pool", reads, writes, lambda: self.nc.gpsimd.indirect_dma_start(
            out=dst, out_offset=bass.IndirectOffsetOnAxis(ap=idx_ap, axis=0), in_=src, in_offset=None, **kw))

    def barrier(self, label=None):
        if label is not None:
            MARKS.append((label, dict(self.cnt)))
        for e in self.eng:
            for e2 in self.eng:
                if e2 != e and self.cnt[e2] > 0:
                    self._wait(e, (e2, self.cnt[e2]))
            for i in range(len(self.dsem)):
                if self.dcnt[i] > 0:
                    self._wait(e, (i, self.dcnt[i]))

    def finish(self, e="sp"):
        for e2 in self.eng:
            if e2 != e and self.cnt[e2] > 0:
                self._wait(e, (e2, self.cnt[e2]))
        for i in range(len(self.dsem)):
            if self.dcnt[i] > 0:
                self._wait(e, (i, self.dcnt[i]))


MARKS = []


class _Stop(Exception):
    pass


class Ring:
    def __init__(self, items):
        self.items = items
        self.i = 0

    def next(self):
        x = self.items[self.i]
        self.i = (self.i + 1) % len(self.items)
        return x


def _bucket(n):
    n = np.maximum(n, 0)
    nf = np.maximum(n, 1).astype(np.float32)
    large = 16 + (np.log(nf / np.float32(16)) / np.float32(math.log(128 / 16)) * np.float32(16)).astype(np.int32)
    large = np.minimum(large, 31)
    return np.where(n < 16, n, large)


def _static_consts(NB):
    c = {}
    c["c_ident"] = np.eye(128, dtype=np.float32)
    oh = np.zeros((33, KF), np.float32)
    k = np.arange(LW); dist = k - 128
    valid = (dist >= 0) & (dist < 512)
    oh[_bucket(dist)[valid], k[valid]] = 1.0; oh[32, k[~valid]] = 1.0
    k = np.arange(LS); dist = k - 128
    valid = dist >= 0
    oh[_bucket(dist)[valid], LW + k[valid]] = 1.0; oh[32, LW + k[~valid]] = 1.0
    k = np.arange(LC); dist = k - 2048
    valid = dist >= 15
    oh[_bucket(dist)[valid], LW + LS + k[valid]] = 1.0; oh[32, LW + LS + k[~valid]] = 1.0
    c["c_oh"] = oh
    E = np.zeros((32, T), np.float32)
    E[np.arange(T) // 64, np.arange(T)] = 1.0
    c["c_E"] = E
    cs = np.arange(128) * 16; ce = cs + 31
    ss = np.arange(32) * 64
    ov = ((cs[:, None] <= ss[None, :] + 63) & (ce[:, None] >= ss[None, :])).astype(np.float32)
    ov[127] = 0.0
    c["c_ov"] = ov
    t = np.arange(T); cur = t // 64; j = np.arange(32)[None, :]
    forced = (j == 0) | (j == cur[:, None]) | (j == cur[:, None] - 1)
    future = j > cur[:, None]
    keep = (~(forced | future)).astype(np.float32)
    addm = np.where(forced, 1e9, np.where(future, -1e9, 0.0)).astype(np.float32)
    c["c_keep"] = np.ascontiguousarray(keep.reshape(NT, 128, 32).transpose(1, 0, 2).reshape(128, NT * 32))
    c["c_addm"] = np.ascontiguousarray(addm.reshape(NT, 128, 32).transpose(1, 0, 2).reshape(128, NT * 32))
    a = np.arange(128)
    c["c_ltri"] = (a[:, None] < a[None, :]).astype(np.float32)
    c["c_ec"] = np.broadcast_to(np.arange(32).astype(np.float32)[None], (128, 32)).copy()
    nblk = NB * T * 2 // BS + NEXP
    c["c_blk0"] = np.broadcast_to((np.arange(nblk) * BS).astype(np.float32)[None], (128, nblk)).copy()
    c["c_tokid"] = (np.arange(NB * NT)[None, :] * 128 + a[:, None]).astype(np.float32)
    nslot = nblk * BS
    si = np.zeros((nslot, 2), np.float32)
    si[:, 0] = NB * T + (np.arange(nslot) % BS) // (BS // 128)
    c["c_slotinit"] = si.reshape(128, -1)
    return c


def _pcol(v, k):
    return np.ascontiguousarray(np.asarray(v, np.float32).reshape(k, 128).T)


NV = 96


def _pack_vecs(inp):
    v = np.zeros((128, NV), np.float32)
    v[:, 0:8] = _pcol(inp["norm_mix"][0], 8)
    v[:, 8:16] = _pcol(inp["norm_x"][0], 8)
    v[:, 16:24] = _pcol(inp["norm_mem"][0], 8)
    v[:, 24:28] = _pcol(inp["out_g_rg"][0], 4)
    v[:, 28:32] = _pcol(inp["out_g_nsa"][0], 4)
    cw = np.asarray(inp["rg_conv_w"][0]).reshape(4, 512)
    for c in range(4):
        for j in range(4):
            v[:, 32 + c * 4 + j] = cw[j, c * 128:(c + 1) * 128]
    v[:, 48:52] = _pcol(inp["rg_conv_b"][0], 4)
    v[:, 52:56] = _pcol(inp["rg_b_r"][0], 4)
    v[:, 56:60] = _pcol(inp["rg_b_i"][0], 4)
    v[:, 60:64] = _pcol(inp["rg_lambda"][0], 4)
    v[:, 64] = np.tile(np.asarray(inp["nsa_g_q"][0]), 2)
    v[:, 65] = np.tile(np.asarray(inp["nsa_g_ks"][0]), 2)
    v[:, 66] = np.tile(np.asarray(inp["nsa_g_kw"][0]), 2)
    v[:, 67] = np.tile(np.asarray(inp["nsa_g_kc"][0]), 2)
    v[:, 68:70] = _pcol(inp["xa_g_q"][0], 2)
    v[:, 70:72] = _pcol(inp["xa_g_k"][0], 2)
    return v


def _bd(w):
    w = np.asarray(w, np.float32)
    o = np.zeros((128, 4, 128), np.float32)
    for c in range(4):
        o[0:64, c, 0:64] = w[2 * c]
        o[64:128, c, 64:128] = w[2 * c + 1]
    return o


def build(NB=4, dbg=(), stop_after=None):
    nc = bass.Bass("TRN2", target_bir_lowering=False)
    NTOK = NB * T
    NBLK = NTOK * 2 // BS + NEXP
    NSLOT = NBLK * BS
    ins = {}

    def din(name, shape, dt=F32):
        ins[name] = nc.dram_tensor(name, list(shape), dt, kind="ExternalInput")
        return ins[name]

    x_d = din("x", [NTOK, D]); mem_d = din("mem", [NB * ML, D])
    relb_d = din("rel_bias", [32, 8]); win_d = din("w_in", [D, PC])
    vecs_d = din("vecs", [128, NV]); gmoe_d = din("gmoe_b", [128, D]); brc_d = din("br_b", [128, 36])
    wbdr_d = din("wbd_r", [128, 4, 128]); wbdi_d = din("wbd_i", [128, 4, 128])
    posk_d = din("posT_k", [64, 32]); posv_d = din("posT_v", [64, 32])
    ckw1_d = din("ck_w1", [2048, 256]); ckw2_d = din("ck_w2", [256, 64])
    cvw1_d = din("cv_w1", [2048, 256]); cvw2_d = din("cv_w2", [256, 64])
    wout_d = din("w_out", [D, D]); xwq_d = din("xa_wq", [D, D]); xwkv_d = din("xa_wkv", [D, 2 * D]); xwo_d = din("xa_wo", [D, D])
    wr_d = din("wr_cat", [D, 36])
    ew1_d = din("exp_w1", [NEXP, D, 512]); ew3_d = din("exp_w3", [NEXP, D, 512]); ew2_d = din("exp_w2", [NEXP, 512, D])
    cst = _static_consts(NB)
    cd = {k: din(k, v.shape) for k, v in cst.items()}
    out_d = nc.dram_tensor("out", [NTOK + 128, D], F32, kind="ExternalOutput")
    FW_d = nc.dram_tensor("FW", [8, 128, LW], F32, kind="Internal")
    FS_d = nc.dram_tensor("FS", [8, 128, LS], F32, kind="Internal")
    FC_d = nc.dram_tensor("FC", [8, 128, LC], F32, kind="Internal")
    CB_d = nc.dram_tensor("CB", [2 * NT, 128, 512], BF16, kind="Internal")
    H1_d = nc.dram_tensor("H1", [NTOK, D], F32, kind="Internal")
    XM_d = nc.dram_tensor("XM", [NTOK + 128, D], BF16, kind="Internal")
    SLOT_d = nc.dram_tensor("SLOT", [NSLOT, 2], F32, kind="Internal")
    EWB_d = [nc.dram_tensor("EWB%d" % i, [NEXP * 128, 4096], BF16, kind="Internal") for i in range(3)]
    dbg_out = {}

    def ddbg(name, shape, dt=F32):
        dbg_out[name] = nc.dram_tensor("dbg_" + name, list(shape), dt, kind="ExternalOutput")
        return dbg_out[name]

    with ExitStack() as es:
        S = Sched(nc, es)
        op = S.op

        def _on_exit(et, ev, tb_):
            if et is _Stop:
                S.finish("sp")
                return True
            return False

        es.push(_on_exit)

        def stop_here(tag):
            if stop_after == tag:
                S.barrier()
                raise _Stop()

        uid = [0]

        def sbt(stack, name, shape, dt):
            uid[0] += 1
            return TB(stack.enter_context(nc.sbuf_tensor("%s_%d" % (name, uid[0]), list(shape), dt)), name)

        psb = [TB(es.enter_context(nc.psum_tensor("ps%d" % i, [128, 512], F32)), "ps%d" % i) for i in range(8)]
        for p_ in psb:
            p_.b.x = True
        PS_A = Ring(psb[0:3])
        PS_B = Ring(psb[3:6])
        PS_C = Ring(psb[6:8])

        ident_f = sbt(es, "ident_f", [128, 128], F32)
        ident_b = sbt(es, "ident_b", [128, 128], BF16)
        ones_b = sbt(es, "ones_b", [128, 128], BF16)
        onesbd_b = sbt(es, "onesbd_b", [128, 128], BF16)
        ltri_b = sbt(es, "ltri_b", [128, 128], BF16)
        E_b = sbt(es, "E_b", [32, T], BF16)
        ov_b = sbt(es, "ov_b", [128, 32], BF16)
        keep_t = sbt(es, "keep_t", [128, NT, 32], F32)
        addm_t = sbt(es, "addm_t", [128, NT, 32], F32)
        ec_t = sbt(es, "ec_t", [128, 32], F32)
        tokid_t = sbt(es, "tokid_t", [128, NB * NT], F32)
        vec = sbt(es, "vec", [128, NV], F32)
        gmoe_t = sbt(es, "gmoe_t", [128, D], F32)
        brc_t = sbt(es, "brc_t", [128, 36], F32)
        wbdr = sbt(es, "wbdr", [128, 4, 128], BF16)
        wbdi = sbt(es, "wbdi", [128, 4, 128], BF16)
        cbk = sbt(es, "cbk", [128, 4], F32)
        WB = sbt(es, "WB", [128, 2, 5, 512], BF16)
        SB = sbt(es, "SB", [128, 2, 3, 512], BF16)
        wr_t = sbt(es, "wr_t", [128, 8, 36], F32)
        carry = sbt(es, "carry", [128, 32], F32)
        blk0_t = sbt(es, "blk0_t", [128, NBLK], F32)
        REC = sbt(es, "REC", [128, NB * NT * 2, 2], F32)
        POSA = sbt(es, "POSA", [128, NB * NT * 2], F32)
        EXPA = sbt(es, "EXPA", [128, NB * NT * 2], F32)
        V_ = lambda a, b=None: vec.t[:, a:(a + 1 if b is None else b)]

        ld = lambda tb, src: S.dma("sp", tb.t[:], src, writes=[tb])
        ldc = lambda tb, src: S.dma("pool", tb.t[:], src, writes=[tb])
        ld(ident_f, cd["c_ident"].ap()[:, :]); ldc(ident_b, cd["c_ident"].ap()[:, :])
        ldc(ltri_b, cd["c_ltri"].ap()[:, :]); ldc(E_b, cd["c_E"].ap()[:, :]); ldc(ov_b, cd["c_ov"].ap()[:, :])
        S.dma("sp", keep_t.t[:].rearrange("p a b -> p (a b)"), cd["c_keep"].ap()[:, :], writes=[keep_t])
        S.dma("sp", addm_t.t[:].rearrange("p a b -> p (a b)"), cd["c_addm"].ap()[:, :], writes=[addm_t])
        ld(ec_t, cd["c_ec"].ap()[:, :]); ld(tokid_t, cd["c_tokid"].ap()[:, :]); ld(blk0_t, cd["c_blk0"].ap()[:, :])
        ld(vec, vecs_d.ap()[:, :]); ld(gmoe_t, gmoe_d.ap()[:, :]); ld(brc_t, brc_d.ap()[:, :])
        ldc(wbdr, wbdr_d.ap()[:, :, :]); ldc(wbdi, wbdi_d.ap()[:, :, :])
        S.dma("sp", wr_t.t[:], wr_d.ap().rearrange("(k p) n -> p k n", p=128), writes=[wr_t])
        op("pool", lambda p: p.memset(ones_b.t[:], 1.0), writes=[ones_b])
        op("pool", lambda p: p.memset(onesbd_b.t[:], 0.0), writes=[onesbd_b])
        op("pool", lambda p: p.memset(onesbd_b.t[0:64, 0:64], 1.0), writes=[onesbd_b])
        op("pool", lambda p: p.memset(onesbd_b.t[64:128, 64:128], 1.0), writes=[onesbd_b])
        op("pool", lambda p: p.memset(carry.t[:], 0.0), writes=[carry])
        op("dve", lambda v: v.memset(V_(84), 1e-6), reads=[vec], writes=[vec])
        op("act", lambda a: a.activation(out=V_(72, 76), in_=V_(60, 64), func=AF.Exp, scale=-1.0), reads=[vec], writes=[vec])
        op("act", lambda a: a.activation(out=V_(72, 76), in_=V_(72, 76), func=AF.Ln, bias=1.0), reads=[vec], writes=[vec])
        op("dve", lambda v: v.tensor_scalar(out=V_(76, 80), in0=V_(72, 76), scalar1=-16.0, scalar2=None, op0=ALU.mult), reads=[vec], writes=[vec])
        op("dve", lambda v: v.tensor_scalar(out=V_(72, 76), in0=V_(72, 76), scalar1=-8.0, scalar2=None, op0=ALU.mult), reads=[vec], writes=[vec])
        op("dve", lambda v: v.tensor_scalar(out=V_(80), in0=V_(64), scalar1=0.125, scalar2=None, op0=ALU.mult), reads=[vec], writes=[vec])
        op("dve", lambda v: v.tensor_scalar(out=V_(81, 83), in0=V_(68, 70), scalar1=1.0 / 16.0, scalar2=None, op0=ALU.mult), reads=[vec], writes=[vec])
        EPS = V_(84)

        stop_here("s0")
        with ExitStack() as st:
            zt = sbt(st, "zt", [128, 2 * D], BF16)
            op("pool", lambda p: p.memset(zt.t[:], 0.0), writes=[zt])
            XMb = Buf("XM"); SLb = Buf("SLOT"); OUTb = Buf("OUT")
            S.dma("sp", XM_d.ap()[NTOK:NTOK + 128, :], zt.t[:, 0:D], reads=[zt], writes=[XMb])
            S.dma("sp", out_d.ap()[NTOK:NTOK + 128, :], zt.t[:].bitcast(F32), reads=[zt], writes=[OUTb])
            sl0 = sbt(st, "sl0", [128, NSLOT // 128 * 2], F32)
            ld(sl0, cd["c_slotinit"].ap()[:, :])
            S.dma("sp", SLOT_d.ap().rearrange("(p a) c -> p (a c)", p=128), sl0.t[:], reads=[sl0], writes=[SLb])
            stop_here("s1")
            tab = sbt(st, "tab", [33, 8], F32)
            op("pool", lambda p: p.memset(tab.t[:], NEG), writes=[tab])
            S.dma("sp", tab.t[0:32, :], relb_d.ap()[:, :], writes=[tab])
            tabrep = sbt(st, "tabrep", [33, 8, 128], F32)
            op("dve", lambda v: v.tensor_copy(out=tabrep.t[:], in_=tab.t[:].unsqueeze(2).to_broadcast([33, 8, 128])), reads=[tab], writes=[tabrep])
            oh = sbt(st, "oh", [33, KF], F32)
            ld(oh, cd["c_oh"].ap()[:, :])
            frow = [sbt(st, "frow%d" % i, [128, KF], F32) for i in range(2)]
            Fb = Buf("F")
            for h in range(8):
                fr = frow[h % 2]
                c0 = 0
                while c0 < KF:
                    n = min(512, KF - c0)
                    ps = PS_A.next()
                    op("pe", lambda t: t.matmul(ps.t[:, 0:n], lhsT=tabrep.t[:, h, :], rhs=oh.t[:, c0:c0 + n], start=True, stop=True), reads=[tabrep, oh], writes=[ps])
                    op("act", lambda a: a.activation(out=fr.t[:, c0:c0 + n], in_=ps.t[:, 0:n], func=AF.Copy), reads=[ps], writes=[fr])
                    c0 += n
                S.dma("sp", FW_d.ap()[h], fr.t[:, 0:LW], reads=[fr], writes=[Fb])
                S.dma("sp", FS_d.ap()[h], fr.t[:, LW:LW + LS], reads=[fr], writes=[Fb])
                S.dma("sp", FC_d.ap()[h], fr.t[:, LW + LS:KF], reads=[fr], writes=[Fb])

            stop_here("s2")

            def toep_load(dst_ap, Fd, L, pstride, off, g, reads, writes):
                hs = 128 * L
                src_ = bass.AP(tensor=Fd, offset=4 * g * hs + off, ap=[[pstride, 128], [hs, 4], [1, 128]])
                S.dma("pool", dst_ap.rearrange("p (b t) -> p b t", b=4), src_, reads=reads, writes=writes)

            for g in range(2):
                for di in range(5):
                    toep_load(WB.t[:, g, di, :], FW_d, LW, LW - 1, di * 128 + 128, g, [Fb], [WB])
                for di in range(3):
                    toep_load(SB.t[:, g, di, :], FS_d, LS, LS - 1, di * 128 + 128, g, [Fb], [SB])
            stop_here("s3")
            cbs = [sbt(st, "cbs%d" % i, [128, 512], BF16) for i in range(2)]
            CBb = Buf("CB")
            for g in range(2):
                for qt in range(NT):
                    cb_ = cbs[(g * NT + qt) % 2]
                    toep_load(cb_.t[:], FC_d, LC, LC - 16, 128 * qt - 16 + 2048, g, [Fb], [cb_])
                    S.dma("sp", CB_d.ap()[g * NT + qt], cb_.t[:], reads=[cb_], writes=[CBb])
            stop_here("s4")
            w1s = sbt(st, "w1s", [64, 32, 256], BF16)
            posb = sbt(st, "posb", [64, 32], BF16)
            for ti, (w1d, pd) in enumerate(((ckw1_d, posk_d), (cvw1_d, posv_d))):
                S.dma("pool", w1s.t[:], w1d.ap().rearrange("(l d) f -> d l f", d=64), writes=[w1s])
                S.dma("pool", posb.t[:], pd.ap()[:, :], writes=[posb])
                for fc in range(2):
                    ps = PS_C.next()
                    for l in range(32):
                        op("pe", lambda t: t.matmul(ps.t[:, 0:1], lhsT=w1s.t[:, l, fc * 128:(fc + 1) * 128], rhs=posb.t[:, l:l + 1], start=(l == 0), stop=(l == 31)), reads=[w1s, posb], writes=[ps])
                    op("act", lambda a: a.activation(out=cbk.t[:, ti * 2 + fc:ti * 2 + fc + 1], in_=ps.t[:, 0:1], func=AF.Copy), reads=[ps], writes=[cbk])
            S.barrier("setup")

        stop_here("s5")
        if "setup" in dbg:
            d1 = ddbg("WB", [128, 2 * 5 * 512]); d2 = ddbg("cbk", [128, 4]); d3 = ddbg("vec", [128, NV])
            with ExitStack() as st:
                tmp = sbt(st, "dbgtmp", [128, 2 * 5 * 512], F32)
                op("dve", lambda v: v.tensor_copy(out=tmp.t[:], in_=WB.t[:].rearrange("p a b c -> p (a b c)")), reads=[WB], writes=[tmp])
                S.dma("sp", d1.ap()[:, :], tmp.t[:], reads=[tmp])
                S.dma("sp", d2.ap()[:, :], cbk.t[:], reads=[cbk])
                S.dma("sp", d3.ap()[:, :], vec.t[:], reads=[vec])
                S.barrier()

        def rstd_inplace(tb, ap, n, extra_reads=()):
            op("act", lambda a: a.activation(out=ap, in_=ap, func=AF.Ln, scale=1.0 / n, bias=EPS[0:ap.shape[0], :]), reads=[tb, vec] + list(extra_reads), writes=[tb])
            op("act", lambda a: a.activation(out=ap, in_=ap, func=AF.Exp, scale=-0.5), reads=[tb], writes=[tb])

        def norm_transpose(src_tb, src_ap, dstT, col0, gcol, scr, ssb, ssi):
            junk, xs16 = scr
            op("act", lambda a: a.activation(out=junk.t[:], in_=src_ap, func=AF.Square, accum_out=ssb.t[:, ssi:ssi + 1]), reads=[src_tb], writes=[junk, ssb])
            rstd_inplace(ssb, ssb.t[:, ssi:ssi + 1], D)
            op("act", lambda a: a.activation(out=xs16.t[:], in_=src_ap, func=AF.Copy, scale=ssb.t[:, ssi:ssi + 1]), reads=[src_tb, ssb], writes=[xs16])
            ps = PS_C.next()
            psv = ps.t[:].bitcast(BF16)
            for kc in range(8):
                op("pe", lambda t: t.transpose(out=psv[:, kc * 128:(kc + 1) * 128], in_=xs16.t[:, kc * 128:(kc + 1) * 128], identity=ident_b.t[:]), reads=[xs16, ident_b], writes=[ps])
            op("dve", lambda v: v.tensor_tensor(out=dstT.t[:, :, col0:col0 + 128], in0=psv.rearrange("p (k t) -> p k t", k=8),
                                                in1=V_(gcol, gcol + 8).unsqueeze(2).to_broadcast([128, 8, 128]), op=ALU.mult), reads=[ps, vec], writes=[dstT])

        moe_bufs = dict(XMb=XMb, SLb=SLb, OUTb=OUTb)
        H1b = Buf("H1")

        for b in range(NB):
            with ExitStack() as eb:
                xT = sbt(eb, "xT", [128, 8, T], BF16)
                mixT = sbt(eb, "mixT", [128, 8, T], BF16)
                with ExitStack() as ea:
                    xs = [sbt(ea, "xs%d" % i, [128, D], F32) for i in range(2)]
                    junk = sbt(ea, "junk", [128, D], BF16)
                    xs16 = sbt(ea, "xs16", [128, D], BF16)
                    ssx = sbt(ea, "ssx", [128, NT], F32)
                    op("dve", lambda v: v.memset(ssx.t[:], 0.0), writes=[ssx])
                    for i in range(NT):
                        xt_ = xs[i % 2]
                        S.dma("sp", xt_.t[:], x_d.ap()[b * T + i * 128: b * T + (i + 1) * 128, :], writes=[xt_])
                        norm_transpose(xt_, xt_.t[:], xT, i * 128, 0, (junk, xs16), ssx, i)
                    if "xT" in dbg and b == 0:
                        S.dma("sp", ddbg("xT", [128, 8 * T], BF16).ap()[:, :], xT.t[:].rearrange("p k t -> p (k t)"), reads=[xT])
                    stop_here("n%d" % len(S.__dict__.setdefault("_nw", [])))
                    S._nw.append(1)
                    wst = [sbt(ea, "wst%d" % i, [128, 8, 512], BF16) for i in range(2)]
                    wring = Ring(wst)
                    winv = win_d.ap().rearrange("(k p) n -> p k n", p=128)

                    def load_w(c0, n, dup64=False):
                        w = wring.next()
                        if dup64:
                            S.dma("pool", w.t[:, :, 0:64], winv[:, :, c0:c0 + 64], writes=[w])
                            S.dma("pool", w.t[:, :, 64:128], winv[:, :, c0:c0 + 64], writes=[w])
                        else:
                            S.dma("pool", w.t[:, :, 0:n], winv[:, :, c0:c0 + n], writes=[w])
                        return w

                    def proj_fm(w, wc0, tb, nco=128):
                        ps = PS_A.next()
                        for kc in range(8):
                            op("pe", lambda t: t.matmul(ps.t[0:nco, :], lhsT=w.t[:, kc, wc0:wc0 + nco], rhs=xT.t[:, kc, tb * 512:(tb + 1) * 512], start=(kc == 0), stop=(kc == 7)), reads=[w, xT], writes=[ps])
                        return ps
                    with ExitStack() as er:
                        B0 = sbt(er, "B0", [128, T + 4], F32)
                        B1 = sbt(er, "B1", [128, T], F32)
                        B2 = sbt(er, "B2", [128, T], BF16)
                        B3 = sbt(er, "B3", [128, T], F32)
                        B4 = sbt(er, "B4", [128, T], F32)
                        B5 = sbt(er, "B5", [128, T], F32)
                        ssa = sbt(er, "ssa", [128, T], F32)
                        TBK = lambda tb: slice(tb * 512, (tb + 1) * 512)
                        for c in range(4):
                            w = load_w(c * 128, 128)
                            op("pool", lambda p: p.memset(B0.t[:, 0:4], 0.0), writes=[B0])
                            for tb in range(4):
                                ps = proj_fm(w, 0, tb)
                                op("act", lambda a: a.activation(out=B0.t[:, 4 + tb * 512: 4 + (tb + 1) * 512], in_=ps.t[:], func=AF.Copy), reads=[ps], writes=[B0])
                            cw = lambda j: V_(32 + c * 4 + j)
                            op("dve", lambda v: v.tensor_scalar(out=B1.t[:], in0=B0.t[:, 4:4 + T], scalar1=cw(3), scalar2=V_(48 + c), op0=ALU.mult, op1=ALU.add), reads=[B0, vec], writes=[B1])
                            for j in (2, 1, 0):
                                op("dve", lambda v: v.scalar_tensor_tensor(out=B1.t[:], in0=B0.t[:, 1 + j:1 + j + T], scalar=cw(j), in1=B1.t[:], op0=ALU.mult, op1=ALU.add), reads=[B0, B1, vec], writes=[B1])
                            op("act", lambda a: a.activation(out=B2.t[:], in_=B1.t[:], func=AF.Copy), reads=[B1], writes=[B2])
                            for tb in range(4):
                                ps = PS_A.next()
                                op("pe", lambda t: t.matmul(ps.t[:], lhsT=wbdr.t[:, c, :], rhs=B2.t[:, TBK(tb)], start=True, stop=True), reads=[wbdr, B2], writes=[ps])
                                op("act", lambda a: a.activation(out=B3.t[:, TBK(tb)], in_=ps.t[:], func=AF.Sigmoid, bias=V_(52 + c)), reads=[ps, vec], writes=[B3])
                                ps = PS_A.next()
                                op("pe", lambda t: t.matmul(ps.t[:], lhsT=wbdi.t[:, c, :], rhs=B2.t[:, TBK(tb)], start=True, stop=True), reads=[wbdi, B2], writes=[ps])
                                op("act", lambda a: a.activation(out=B4.t[:, TBK(tb)], in_=ps.t[:], func=AF.Sigmoid, bias=V_(56 + c)), reads=[ps, vec], writes=[B4])
                            op("act", lambda a: a.activation(out=B5.t[:], in_=B3.t[:], func=AF.Exp, scale=V_(72 + c)), reads=[B3, vec], writes=[B5])
                            op("act", lambda a: a.activation(out=B3.t[:], in_=B3.t[:], func=AF.Exp, scale=V_(76 + c)), reads=[B3, vec], writes=[B3])
                            op("act", lambda a: a.activation(out=B3.t[:], in_=B3.t[:], func=AF.Sqrt, scale=-1.0, bias=1.0), reads=[B3], writes=[B3])
                            op("dve", lambda v: v.tensor_tensor(out=B3.t[:], in0=B3.t[:], in1=B4.t[:], op=ALU.mult), reads=[B3, B4], writes=[B3])
                            op("dve", lambda v: v.tensor_tensor(out=B3.t[:], in0=B3.t[:], in1=B1.t[:], op=ALU.mult), reads=[B3, B1], writes=[B3])
                            op("dve", lambda v: v.tensor_tensor_scan(out=B4.t[:], data0=B5.t[:], data1=B3.t[:], initial=0.0, op0=ALU.mult, op1=ALU.add), reads=[B5, B3], writes=[B4])
                            w = load_w(512 + c * 128, 128)
                            for tb in range(4):
                                ps = proj_fm(w, 0, tb)
                                op("act", lambda a: a.activation(out=B0.t[:, TBK(tb)], in_=ps.t[:], func=AF.Gelu), reads=[ps], writes=[B0])
                            op("dve", lambda v: v.tensor_tensor(out=B1.t[:], in0=B0.t[:, 0:T], in1=B4.t[:], op=ALU.mult), reads=[B0, B4], writes=[B1])
                            op("act", lambda a: a.activation(out=mixT.t[:, c, :], in_=B1.t[:], func=AF.Copy), reads=[B1], writes=[mixT])
                            op("act", lambda a: a.activation(out=B2.t[:], in_=B1.t[:], func=AF.Square), reads=[B1], writes=[B2])
                            for tb in range(4):
                                ps = PS_B.next()
                                op("pe", lambda t: t.matmul(ps.t[:], lhsT=ones_b.t[:], rhs=B2.t[:, TBK(tb)], start=True, stop=True), reads=[ones_b, B2], writes=[ps])
                                if c == 0:
                                    op("dve", lambda v: v.tensor_copy(out=ssa.t[:, TBK(tb)], in_=ps.t[:]), reads=[ps], writes=[ssa])
                                else:
                                    op("dve", lambda v: v.tensor_tensor(out=ssa.t[:, TBK(tb)], in0=ssa.t[:, TBK(tb)], in1=ps.t[:], op=ALU.add), reads=[ps, ssa], writes=[ssa])
                        rstd_inplace(ssa, ssa.t[:], 512)
                        for c in range(4):
                            op("dve", lambda v: v.scalar_tensor_tensor(out=mixT.t[:, c, :], in0=mixT.t[:, c, :], scalar=V_(24 + c), in1=ssa.t[:], op0=ALU.mult, op1=ALU.mult), reads=[mixT, ssa, vec], writes=[mixT])
                    S.barrier("A1")
                if "rg1" in dbg and b == 0:
                    S.dma("sp", ddbg("mixT", [128, 8 * T], BF16).ap()[:, :], mixT.t[:].rearrange("p a b -> p (a b)"), reads=[mixT])
                    S.barrier()
                eb1 = ExitStack()
                qT = sbt(eb1, "qT", [128, 8, T], BF16)
                ksT = sbt(eb1, "ksT", [128, 2, T], BF16)
                kwT = sbt(eb1, "kwT", [128, 2, T], BF16)
                Vs = sbt(eb1, "Vs", [128, NT, 2, 65], BF16)
                Vw = sbt(eb1, "Vw", [128, NT, 2, 65], BF16)
                gates = sbt(eb1, "gates", [128, NT, 24], F32)
                kcmpT = sbt(eb1, "kcmpT", [128, 2, 128], BF16)
                vcmp = sbt(eb1, "vcmp", [128, 2, 97], BF16)
                op("pool", lambda p: p.memset(Vs.t[:, :, :, 64:65], 1.0), writes=[Vs])
                op("pool", lambda p: p.memset(Vw.t[:, :, :, 64:65], 1.0), writes=[Vw])
                op("pool", lambda p: p.memset(kcmpT.t[:], 0.0), writes=[kcmpT])
                qNm = Buf("qNm")
                op("pool", lambda p: p.memset(qT.t[64:128, :, :], 0.0), writes=[qT])
                op("pool", lambda p: p.memset(kwT.t[64:128, :, :], 0.0), writes=[kwT])
                op("pool", lambda p: p.memset(ksT.t[96:128, :, :], 0.0), writes=[ksT])
                for g in range(2):
                    S.dma("pool", ksT.t[64:96, g, :], cd["c_E"].ap()[:, :], writes=[ksT])
                op("pool", lambda p: p.memset(vcmp.t[:], 0.0), writes=[vcmp])
                op("pool", lambda p: p.memset(vcmp.t[:, :, 64:65], 1.0), writes=[vcmp])
                for g in range(2):
                    op("pool", lambda p: p.tensor_copy(out=vcmp.t[:, g, 65:97], in_=ov_b.t[:]), reads=[ov_b], writes=[vcmp])
                with ExitStack() as ea:
                    stop_here("n%d" % len(S.__dict__.setdefault("_nw", [])))
                    S._nw.append(1)
                    wst = [sbt(ea, "wst%d" % i, [128, 8, 512], BF16) for i in range(2)]
                    wring = Ring(wst)
                    winv = win_d.ap().rearrange("(k p) n -> p k n", p=128)

                    def load_w(c0, n, dup64=False):
                        w = wring.next()
                        if dup64:
                            S.dma("pool", w.t[:, :, 0:64], winv[:, :, c0:c0 + 64], writes=[w])
                            S.dma("pool", w.t[:, :, 64:128], winv[:, :, c0:c0 + 64], writes=[w])
                        else:
                            S.dma("pool", w.t[:, :, 0:n], winv[:, :, c0:c0 + n], writes=[w])
                        return w

                    def proj_fm(w, wc0, tb, nco=128):
                        ps = PS_A.next()
                        for kc in range(8):
                            op("pe", lambda t: t.matmul(ps.t[0:nco, :], lhsT=w.t[:, kc, wc0:wc0 + nco], rhs=xT.t[:, kc, tb * 512:(tb + 1) * 512], start=(kc == 0), stop=(kc == 7)), reads=[w, xT], writes=[ps])
                        return ps
                    en2 = ExitStack()
                    q32 = [sbt(en2, "q32_%d" % i, [128, 512], F32) for i in range(2)]
                    qsq = [sbt(en2, "qsq_%d" % i, [128, 512], BF16) for i in range(2)]
                    rsn = [sbt(en2, "rsn_%d" % i, [128, 512], F32) for i in range(2)]
                    nrm_i = [0]

                    hn_pend = []

                    def hn_flush():
                        while hn_pend:
                            a32, asq, ars, dst_ap, dst_tb, gcol = hn_pend.pop(0)
                            p2 = PS_B.next()
                            op("pe", lambda t: t.matmul(p2.t[0:64, :], lhsT=ones_b.t[0:64, 0:64], rhs=asq.t[0:64, :], start=True, stop=True), reads=[ones_b, asq], writes=[p2])
                            op("act", lambda a: a.activation(out=ars.t[0:64, :], in_=p2.t[0:64, :], func=AF.Ln, scale=1.0 / 64, bias=EPS[0:64, :]), reads=[p2, vec], writes=[ars])
                            op("act", lambda a: a.activation(out=ars.t[0:64, :], in_=ars.t[0:64, :], func=AF.Exp, scale=-0.5), reads=[ars], writes=[ars])
                            op("dve", lambda v: v.scalar_tensor_tensor(out=dst_ap, in0=a32.t[0:64, :], scalar=vec.t[0:64, gcol:gcol + 1], in1=ars.t[0:64, :], op0=ALU.mult, op1=ALU.mult), reads=[a32, ars, vec], writes=[dst_tb])

                    def headnorm_store(ps, dst_ap, dst_tb, gcol):
                        hn_flush()
                        k = nrm_i[0] % 2; nrm_i[0] += 1
                        a32, asq, ars = q32[k], qsq[k], rsn[k]
                        op("act", lambda a: a.activation(out=asq.t[0:64, :], in_=ps.t[0:64, :], func=AF.Square), reads=[ps], writes=[asq])
                        op("dve", lambda v: v.tensor_copy(out=a32.t[0:64, :], in_=ps.t[0:64, :]), reads=[ps], writes=[a32])
                        hn_pend.append((a32, asq, ars, dst_ap, dst_tb, gcol))

                    w = load_w(1024, 512)
                    for h in range(8):
                        for tb in range(4):
                            ps = proj_fm(w, h * 64, tb, 64)
                            headnorm_store(ps, qT.t[0:64, h, tb * 512:(tb + 1) * 512], qT, 80)
                    stop_here("a2q")
                    for (c0, dstk, gcol) in ((1792, ksT, 65), (2048, kwT, 66)):
                        w = load_w(c0, 128)
                        for g in range(2):
                            for tb in range(4):
                                ps = proj_fm(w, g * 64, tb, 64)
                                headnorm_store(ps, dstk.t[0:64, g, tb * 512:(tb + 1) * 512], dstk, gcol)
                    hn_flush()
                    S.barrier()
                    en2.close()
                    stop_here("a2k")
                    kcT = sbt(ea, "kcT", [128, T], BF16)
                    vcT = sbt(ea, "vcT", [128, T], BF16)
                    w = load_w(1536, 256)
                    for ci, dst in enumerate((kcT, vcT)):
                        for tb in range(4):
                            ps = proj_fm(w, ci * 128, tb)
                            op("act", lambda a: a.activation(out=dst.t[:, tb * 512:(tb + 1) * 512], in_=ps.t[:], func=AF.Copy), reads=[ps], writes=[dst])
                    stop_here("a2c")
                    w = load_w(1920, 408)
                    for i in range(NT):
                        ps = PS_A.next()
                        for kc in range(8):
                            op("pe", lambda t: t.matmul(ps.t[:, 0:408], lhsT=xT.t[:, kc, i * 128:(i + 1) * 128], rhs=w.t[:, kc, 0:408], start=(kc == 0), stop=(kc == 7)), reads=[w, xT], writes=[ps])
                        op("dve", lambda v: v.tensor_copy(out=Vs.t[:, i, :, 0:64], in_=ps.t[:, 0:128].rearrange("p (g d) -> p g d", g=2)), reads=[ps], writes=[Vs])
                        op("dve", lambda v: v.tensor_copy(out=Vw.t[:, i, :, 0:64], in_=ps.t[:, 256:384].rearrange("p (g d) -> p g d", g=2)), reads=[ps], writes=[Vw])
                        op("act", lambda a: a.activation(out=gates.t[:, i, :], in_=ps.t[:, 384:408], func=AF.Sigmoid), reads=[ps], writes=[gates])
                    stop_here("a2t")
                    with ExitStack() as ec:
                        w1 = sbt(ec, "w1", [128, 32, 256], BF16)
                        w2 = sbt(ec, "w2", [128, 2, 128], BF16)
                        hT = sbt(ec, "hT", [128, 2, 128], BF16)
                        k32 = sbt(ec, "k32", [128, 128], F32)
                        ksq = sbt(ec, "ksq", [128, 128], BF16)
                        krs = sbt(ec, "krs", [128, 128], F32)
                        for ti, (w1d, w2d, srcT) in enumerate(((ckw1_d, ckw2_d, kcT), (cvw1_d, cvw2_d, vcT))):
                            w1v = w1d.ap().rearrange("(l d) f -> d l f", d=64)
                            S.dma("pool", w1.t[0:64], w1v, writes=[w1])
                            S.dma("pool", w1.t[64:128], w1v, writes=[w1])
                            w2v = w2d.ap().rearrange("(c f) d -> f c d", f=128)
                            S.dma("pool", w2.t[:, :, 0:64], w2v, writes=[w2])
                            S.dma("pool", w2.t[:, :, 64:128], w2v, writes=[w2])
                            for g in range(2):
                                pr = slice(g * 64, (g + 1) * 64)
                                ps = PS_A.next()
                                for fc in range(2):
                                    for l in range(32):
                                        op("pe", lambda t: t.matmul(ps.t[:, fc * 128:fc * 128 + 127], lhsT=w1.t[pr, l, fc * 128:(fc + 1) * 128], rhs=srcT.t[pr, l:l + 16 * 126 + 1:16], start=(l == 0), stop=(l == 31)), reads=[w1, srcT], writes=[ps])
                                for fc in range(2):
                                    op("act", lambda a: a.activation(out=hT.t[:, fc, 0:127], in_=ps.t[:, fc * 128:fc * 128 + 127], func=AF.Gelu, bias=cbk.t[:, ti * 2 + fc:ti * 2 + fc + 1]), reads=[ps, cbk], writes=[hT])
                                p2 = PS_B.next()
                                if ti == 0:
                                    for fc in range(2):
                                        op("pe", lambda t: t.matmul(p2.t[:, 0:127], lhsT=w2.t[:, fc, :], rhs=hT.t[:, fc, 0:127], start=(fc == 0), stop=(fc == 1)), reads=[w2, hT], writes=[p2])
                                    op("act", lambda a: a.activation(out=ksq.t[:, 0:127], in_=p2.t[:, 0:127], func=AF.Square), reads=[p2], writes=[ksq])
                                    op("dve", lambda v: v.tensor_copy(out=k32.t[:, 0:127], in_=p2.t[:, 0:127]), reads=[p2], writes=[k32])
                                    p3 = PS_B.next()
                                    op("pe", lambda t: t.matmul(p3.t[:, 0:127], lhsT=onesbd_b.t[:], rhs=ksq.t[:, 0:127], start=True, stop=True), reads=[onesbd_b, ksq], writes=[p3])
                                    op("act", lambda a: a.activation(out=krs.t[:, 0:127], in_=p3.t[:, 0:127], func=AF.Ln, scale=1.0 / 64, bias=EPS), reads=[p3, vec], writes=[krs])
                                    op("act", lambda a: a.activation(out=krs.t[:, 0:127], in_=krs.t[:, 0:127], func=AF.Exp, scale=-0.5), reads=[krs], writes=[krs])
                                    op("dve", lambda v: v.scalar_tensor_tensor(out=kcmpT.t[0:64, g, 0:127], in0=k32.t[0:64, 0:127], scalar=vec.t[0:64, 67:68], in1=krs.t[0:64, 0:127], op0=ALU.mult, op1=ALU.mult), reads=[k32, krs, vec], writes=[kcmpT])
                                else:
                                    for fc in range(2):
                                        op("pe", lambda t: t.matmul(p2.t[0:127, 0:64], lhsT=hT.t[:, fc, 0:127], rhs=w2.t[:, fc, 0:64], start=(fc == 0), stop=(fc == 1)), reads=[w2, hT], writes=[p2])
                                    op("act", lambda a: a.activation(out=vcmp.t[0:127, g, 0:64], in_=p2.t[0:127, 0:64], func=AF.Copy), reads=[p2], writes=[vcmp])
                    if "cmp" in dbg and b == 0:
                        S.dma("sp", ddbg("kcmpT", [64, 256], BF16).ap()[:, :], kcmpT.t[:].rearrange("p a b -> p (a b)"), reads=[kcmpT])
                        S.dma("sp", ddbg("vcmp", [128, 2 * 97], BF16).ap()[:, :], vcmp.t[:].rearrange("p a b -> p (a b)"), reads=[vcmp])
                        S.dma("sp", ddbg("qT", [64, 8 * T], BF16).ap()[:, :], qT.t[:].rearrange("p a b -> p (a b)"), reads=[qT])
                        S.dma("sp", ddbg("kwT", [64, 2 * T], BF16).ap()[:, :], kwT.t[:].rearrange("p a b -> p (a b)"), reads=[kwT])
                        S.dma("sp", ddbg("Vw", [128, NT * 2 * 65], BF16).ap()[:, :], Vw.t[:].rearrange("p a b c -> p (a b c)"), reads=[Vw])
                        S.dma("sp", ddbg("gates", [128, NT * 24], F32).ap()[:, :], gates.t[:].rearrange("p a b -> p (a b)"), reads=[gates])

                    S.barrier("A2")
                if "rg" in dbg and b == 0:
                    S.dma("sp", ddbg("mixT", [128, 8 * T], BF16).ap()[:, :], mixT.t[:].rearrange("p a b -> p (a b)"), reads=[mixT])
                if stop_after == "A":
                    S.barrier()
                    eb1.close()
                    continue

                with ExitStack() as eat:
                    ynsa = TB(xT.t[:].rearrange("p k t -> p (k t)").bitcast(F32).rearrange("p (a c) -> p a c", a=NT), "ynsa")
                    ynsa.b = xT.b
                    nmp = [sbt(eat, "nmp%d" % i, [128, 128], BF16) for i in range(2)]
                    for n_ in nmp:
                        op("pool", lambda p: p.memset(n_.t[:], 0.0), writes=[n_])
                    cbt = [sbt(eat, "cbt%d" % i, [128, 512], BF16) for i in range(2)]
                    pTs = Ring([sbt(eat, "pT%d" % i, [128, 512], BF16) for i in range(4)])
                    sm = Ring([sbt(eat, "sm%d" % i, [128, 96], F32) for i in range(4)])
                    otmp = Ring([sbt(eat, "otmp%d" % i, [128, 4, 64], F32) for i in range(3)])
                    itmp = [sbt(eat, "itmp%d" % i, [128, 4, 32], F32) for i in range(2)]
                    imp = [sbt(eat, "imp%d" % i, [128, 32], F32) for i in range(2)]
                    nm = sbt(eat, "nm", [128, 32], F32)

                    def qk_scores(ps, kT, g, kt, qt, first):
                        ks_ = slice(kt * 128, (kt + 1) * 128); qs_ = slice(qt * 128, (qt + 1) * 128)
                        op("pe", lambda t: t.matmul(ps.t[:].rearrange("p (a t) -> p a t", a=4), lhsT=kT[:, g, ks_] if kT is not None else kcmpT.t[:, g, :],
                                                    rhs=qT.t[:, 4 * g:4 * g + 4, qs_], start=False, stop=True), reads=[qT] + first, writes=[ps])

                    def finish_branch(po, ncol, g, qt, br, first):
                        pov = po.t[:, 0:4 * ncol].rearrange("p (s c) -> p s c", s=4)
                        s_ = sm.next()
                        rden = s_.t[:, 0:4]; wv = s_.t[:, 4:8]
                        op("dve", lambda v: v.tensor_scalar(out=rden, in0=pov[:, :, 64], scalar1=1e-30, scalar2=None, op0=ALU.add), reads=[po], writes=[s_])
                        op("dve", lambda v: v.reciprocal(out=rden, in_=rden), reads=[s_], writes=[s_])
                        gv = gates.t[:, qt, 12 * g:12 * g + 12].rearrange("p (s b) -> p s b", s=4)[:, :, br]
                        op("dve", lambda v: v.tensor_tensor(out=wv, in0=rden, in1=gv, op=ALU.mult), reads=[s_, gates], writes=[s_])
                        yv = ynsa.t[:, qt, g * 256:(g + 1) * 256].rearrange("p (s d) -> p s d", s=4)
                        if first:
                            op("dve", lambda v: v.tensor_tensor(out=yv, in0=pov[:, :, 0:64], in1=wv.unsqueeze(2).to_broadcast([128, 4, 64]), op=ALU.mult), reads=[po, s_], writes=[ynsa])
                        else:
                            o_ = otmp.next()
                            op("dve", lambda v: v.tensor_tensor(out=o_.t[:], in0=pov[:, :, 0:64], in1=wv.unsqueeze(2).to_broadcast([128, 4, 64]), op=ALU.mult), reads=[po, s_], writes=[o_])
                            op("pool", lambda p: p.tensor_tensor(out=yv, in0=yv, in1=o_.t[:], op=ALU.add), reads=[o_, ynsa], writes=[ynsa])
                        return s_

                    cmp_units = [(g, qt) for g in range(2) for qt in range(NT)]

                    def cmp_scores(u):
                        g, qt = cmp_units[u]
                        cb_ = cbt[u % 2]
                        S.dma("sp", cb_.t[:], CB_d.ap()[g * NT + qt], reads=[CBb], writes=[cb_])
                        ps = PS_A.next()
                        op("pe", lambda t: t.matmul(ps.t[:], lhsT=ident_b.t[:], rhs=cb_.t[:], start=True, stop=False), reads=[ident_b, cb_], writes=[ps])
                        qk_scores(ps, None, g, 0, qt, [kcmpT])
                        pT = pTs.next()
                        op("act", lambda a: a.activation(out=pT.t[:], in_=ps.t[:], func=AF.Exp), reads=[ps], writes=[pT])
                        return pT

                    def cmp_pv(u, pT):
                        g, qt = cmp_units[u]
                        po = PS_B.next()
                        for sl in range(4):
                            op("pe", lambda t: t.matmul(po.t[:, sl * 97:(sl + 1) * 97], lhsT=pT.t[:, sl * 128:(sl + 1) * 128], rhs=vcmp.t[:, g, :], start=(sl == 0), stop=True, skip_group_check=True), reads=[pT, vcmp], writes=[po])
                        s_ = finish_branch(po, 97, g, qt, 0, True)
                        it_ = itmp[u % 2]; im_ = imp[u % 2]
                        pov = po.t[:, 0:4 * 97].rearrange("p (s c) -> p s c", s=4)
                        op("dve", lambda v: v.tensor_tensor(out=it_.t[:], in0=pov[:, :, 65:97], in1=s_.t[:, 0:4].unsqueeze(2).to_broadcast([128, 4, 32]), op=ALU.mult), reads=[po, s_], writes=[it_])
                        op("dve", lambda v: v.tensor_reduce(out=im_.t[:], in_=it_.t[:].rearrange("p s j -> p j s"), axis=AX.X, op=ALU.add), reads=[it_], writes=[im_])
                        op("dve", lambda v: v.tensor_tensor(out=im_.t[:], in0=im_.t[:], in1=keep_t.t[:, qt, :], op=ALU.mult), reads=[im_, keep_t], writes=[im_])
                        op("dve", lambda v: v.tensor_tensor(out=im_.t[:], in0=im_.t[:], in1=addm_t.t[:, qt, :], op=ALU.add), reads=[im_, addm_t], writes=[im_])
                        op("dve", lambda v: v.max(out=s_.t[:, 8:16], in_=im_.t[:]), reads=[im_], writes=[s_])
                        nm_ = nmp[u % 2]
                        op("dve", lambda v: v.tensor_scalar(out=nm_.t[:, 64:96], in0=im_.t[:], scalar1=s_.t[:, 15:16], scalar2=NEG, op0=ALU.is_lt, op1=ALU.mult), reads=[im_, s_], writes=[nm_])
                        return nm_

                    def cmp_tail(u, nm_):
                        g, qt = cmp_units[u]
                        pt_ = PS_C.next()
                        op("pe", lambda t: t.matmul(pt_.t[:, 0:128], lhsT=nm_.t[:], rhs=ident_b.t[:], start=True, stop=True), reads=[nm_, ident_b], writes=[pt_])
                        op("act", lambda a: a.activation(out=qT.t[64:96, 4 * g:4 * g + 4, qt * 128:(qt + 1) * 128], in_=pt_.t[64:96, 0:128].unsqueeze(1).to_broadcast([32, 4, 128]), func=AF.Copy), reads=[pt_], writes=[qNm])

                    nU = len(cmp_units)
                    pTq = {0: cmp_scores(0)}
                    nmq = {}
                    for u in range(nU):
                        if u + 1 < nU:
                            pTq[u + 1] = cmp_scores(u + 1)
                        nmq[u] = cmp_pv(u, pTq.pop(u))
                        if u >= 1:
                            cmp_tail(u - 1, nmq.pop(u - 1))
                    cmp_tail(nU - 1, nmq.pop(nU - 1))
                    if "nm" in dbg and b == 0:
                        S.dma("sp", ddbg("ynsa_c", [128, NT * 512], F32).ap()[:, :], ynsa.t[:].rearrange("p a b -> p (a b)"), reads=[ynsa])

                    MARKS.append(("Bcmp", dict(S.cnt)))
                    n_cv = (3 * NEXP + NB - 1) // NB
                    for ci in range(b * n_cv, min(3 * NEXP, (b + 1) * n_cv)):
                        wi, e_ = ci % 3, ci // 3
                        src_ = (ew1_d, ew3_d, ew2_d)[wi].ap()[e_].rearrange("(p k) n -> p (k n)", p=128)
                        S.dma("pool", EWB_d[wi].ap()[e_ * 128:(e_ + 1) * 128, :], src_, writes=[Buf()])
                    jobs = []
                    for qt in range(NT):
                        for g in range(2):
                            for kt in range(qt + 1):
                                jobs.append(("sel", g, qt, kt, kt == 0, kt == qt))
                            k0 = max(0, qt - 4)
                            for kt in range(k0, qt + 1):
                                jobs.append(("win", g, qt, kt, kt == k0, kt == qt))
                    pos_ = {}

                    def emit_scores(job):
                        kind, g, qt, kt, first, last = job
                        ps = PS_A.next()
                        if kind == "sel":
                            di = min(qt - kt, 2)
                            op("pe", lambda t: t.matmul(ps.t[:], lhsT=ident_b.t[:], rhs=SB.t[:, g, di, :], start=True, stop=False), reads=[ident_b, SB], writes=[ps])
                            qk_scores(ps, ksT.t, g, kt, qt, [ksT, qNm])
                        else:
                            di = qt - kt
                            op("pe", lambda t: t.matmul(ps.t[:], lhsT=ident_b.t[:], rhs=WB.t[:, g, di, :], start=True, stop=False), reads=[ident_b, WB], writes=[ps])
                            qk_scores(ps, kwT.t, g, kt, qt, [kwT])
                        return ps

                    def emit_rest(job, ps):
                        kind, g, qt, kt, first, last = job
                        pT = pTs.next()
                        op("act", lambda a: a.activation(out=pT.t[:], in_=ps.t[:], func=AF.Exp), reads=[ps], writes=[pT])
                        if first:
                            pos_[(kind, g, qt)] = PS_B.next()
                        po = pos_[(kind, g, qt)]
                        Vt = Vs if kind == "sel" else Vw
                        for sl in range(4):
                            op("pe", lambda t: t.matmul(po.t[:, sl * 65:(sl + 1) * 65], lhsT=pT.t[:, sl * 128:(sl + 1) * 128], rhs=Vt.t[:, kt, g, :], start=(first and sl == 0), stop=last, skip_group_check=True), reads=[pT, Vt], writes=[po])
                        if last:
                            finish_branch(po, 65, g, qt, 1 if kind == "sel" else 2, False)
                            del pos_[(kind, g, qt)]

                    pend = None
                    for job in jobs:
                        ps = emit_scores(job)
                        if pend is not None:
                            emit_rest(*pend)
                        pend = (job, ps)
                    emit_rest(*pend)
                    if "ynsa" in dbg and b == 0:
                        S.dma("sp", ddbg("ynsa", [128, NT * 512], F32).ap()[:, :], ynsa.t[:].rearrange("p a b -> p (a b)"), reads=[ynsa])
                    MARKS.append(("Bsw", dict(S.cnt)))
                    junk2 = sbt(eat, "junk2", [128, 512], BF16)
                    yn16 = sbt(eat, "yn16", [128, 512], BF16)
                    ssy = sbt(eat, "ssy", [128, NT], F32)
                    op("dve", lambda v: v.memset(ssy.t[:], 0.0), writes=[ssy])
                    for qt in range(NT):
                        op("act", lambda a: a.activation(out=junk2.t[:], in_=ynsa.t[:, qt, :], func=AF.Square, accum_out=ssy.t[:, qt:qt + 1]), reads=[ynsa], writes=[junk2, ssy])
                        rstd_inplace(ssy, ssy.t[:, qt:qt + 1], 512)
                        op("act", lambda a: a.activation(out=yn16.t[:], in_=ynsa.t[:, qt, :], func=AF.Copy, scale=ssy.t[:, qt:qt + 1]), reads=[ynsa, ssy], writes=[yn16])
                        ps = PS_C.next()
                        psv = ps.t[:].bitcast(BF16)
                        for cc in range(4):
                            op("pe", lambda t: t.transpose(out=psv[:, cc * 128:(cc + 1) * 128], in_=yn16.t[:, cc * 128:(cc + 1) * 128], identity=ident_b.t[:]), reads=[yn16, ident_b], writes=[ps])
                        op("dve", lambda v: v.tensor_tensor(out=mixT.t[:, 4:8, qt * 128:(qt + 1) * 128], in0=psv[:, 0:512].rearrange("p (k t) -> p k t", k=4),
                                                            in1=V_(28, 32).unsqueeze(2).to_broadcast([128, 4, 128]), op=ALU.mult), reads=[ps, vec], writes=[mixT])
                    S.barrier("B")
                eb1.close()
                if stop_after == "B":
                    S.barrier()
                    continue

                with ExitStack() as ecx:
                    wo = sbt(ecx, "wo", [128, 8, D], BF16)
                    S.dma("pool", wo.t[:], wout_d.ap().rearrange("(k p) n -> p k n", p=128), writes=[wo])
                    xs = [sbt(ecx, "xr%d" % i, [128, D], F32) for i in range(2)]
                    junk = sbt(ecx, "junkc", [128, D], BF16)
                    xs16 = sbt(ecx, "xs16c", [128, D], BF16)
                    ssx = sbt(ecx, "ssxc", [128, NT], F32)
                    op("dve", lambda v: v.memset(ssx.t[:], 0.0), writes=[ssx])
                    for i in range(NT):
                        xt_ = xs[i % 2]
                        rows = slice(b * T + i * 128, b * T + (i + 1) * 128)
                        S.dma("sp", xt_.t[:], x_d.ap()[rows, :], writes=[xt_])
                        for hf in range(2):
                            ps = PS_A.next()
                            for cc in range(8):
                                op("pe", lambda t: t.matmul(ps.t[:], lhsT=mixT.t[:, cc, i * 128:(i + 1) * 128], rhs=wo.t[:, cc, hf * 512:(hf + 1) * 512], start=(cc == 0), stop=(cc == 7)), reads=[mixT, wo], writes=[ps])
                            op("dve", lambda v: v.tensor_tensor(out=xt_.t[:, hf * 512:(hf + 1) * 512], in0=xt_.t[:, hf * 512:(hf + 1) * 512], in1=ps.t[:], op=ALU.add), reads=[ps, xt_], writes=[xt_])
                        S.dma("sp", H1_d.ap()[rows, :], xt_.t[:], reads=[xt_], writes=[Buf()])
                        norm_transpose(xt_, xt_.t[:], xT, i * 128, 8, (junk, xs16), ssx, i)
                    S.barrier("C")
                if "h1" in dbg and b == 0:
                    with ExitStack() as st:
                        tmp = sbt(st, "dbgh1", [128, NT, D], F32)
                        S.dma("sp", tmp.t[:], H1_d.ap()[0:T, :].rearrange("(i p) d -> p i d", p=128), reads=[H1b], writes=[tmp])
                        S.dma("sp", ddbg("h1", [T, D]).ap().rearrange("(i p) d -> p i d", p=128), tmp.t[:], reads=[tmp])
                        S.barrier()
                if stop_after == "C":
                    S.barrier()
                    continue

                with ExitStack() as ed:
                    OT = sbt(ed, "OT", [128, 8, T], BF16)
                    with ExitStack() as ed1:
                        mT = sbt(ed1, "mT", [128, 8, ML], BF16)
                        kxT = sbt(ed1, "kxT", [128, 8, ML], BF16)
                        Vx = sbt(ed1, "Vx", [128, 2, D], BF16)
                        xs = [sbt(ed1, "xm%d" % i, [128, D], F32) for i in range(2)]
                        junk = sbt(ed1, "junkd", [128, D], BF16)
                        xs16 = sbt(ed1, "xs16d", [128, D], BF16)
                        ssx = sbt(ed1, "ssxd", [128, 2], F32)
                        op("dve", lambda v: v.memset(ssx.t[:], 0.0), writes=[ssx])
                        for i in range(2):
                            S.dma("sp", xs[i].t[:], mem_d.ap()[b * ML + i * 128: b * ML + (i + 1) * 128, :], writes=[xs[i]])
                            norm_transpose(xs[i], xs[i].t[:], mT, i * 128, 16, (junk, xs16), ssx, i)
                        wkv = Ring([sbt(ed1, "wkv%d" % i, [128, 8, 512], BF16) for i in range(2)])
                        wkvv = xwkv_d.ap().rearrange("(k p) n -> p k n", p=128)
                        k32 = [sbt(ed1, "kx32_%d" % i, [128, ML], F32) for i in range(2)]
                        ksq = sbt(ed1, "kxsq", [128, ML], BF16)
                        krs = sbt(ed1, "kxrs", [128, ML], F32)
                        for grp in range(2):
                            w = wkv.next()
                            S.dma("pool", w.t[:], wkvv[:, :, grp * 512:(grp + 1) * 512], writes=[w])
                            for hl in range(2):
                                h = grp * 2 + hl
                                pss = PS_B.next()
                                for dc in range(2):
                                    ps = PS_A.next()
                                    for kc in range(8):
                                        op("pe", lambda t: t.matmul(ps.t[:, 0:ML], lhsT=w.t[:, kc, (hl * 2 + dc) * 128:(hl * 2 + dc + 1) * 128], rhs=mT.t[:, kc, :], start=(kc == 0), stop=(kc == 7)), reads=[w, mT], writes=[ps])
                                    op("act", lambda a: a.activation(out=ksq.t[:], in_=ps.t[:, 0:ML], func=AF.Square), reads=[ps], writes=[ksq])
                                    op("dve", lambda v: v.tensor_copy(out=k32[dc].t[:], in_=ps.t[:, 0:ML]), reads=[ps], writes=[k32[dc]])
                                    op("pe", lambda t: t.matmul(pss.t[:, 0:ML], lhsT=ones_b.t[:], rhs=ksq.t[:], start=(dc == 0), stop=(dc == 1)), reads=[ones_b, ksq], writes=[pss])
                                op("act", lambda a: a.activation(out=krs.t[:], in_=pss.t[:, 0:ML], func=AF.Ln, scale=1.0 / 256, bias=EPS), reads=[pss, vec], writes=[krs])
                                op("act", lambda a: a.activation(out=krs.t[:], in_=krs.t[:], func=AF.Exp, scale=-0.5), reads=[krs], writes=[krs])
                                for dc in range(2):
                                    op("dve", lambda v: v.scalar_tensor_tensor(out=kxT.t[:, h * 2 + dc, :], in0=k32[dc].t[:], scalar=V_(70 + dc), in1=krs.t[:], op0=ALU.mult, op1=ALU.mult), reads=[k32[dc], krs, vec], writes=[kxT])
                        for hf in range(2):
                            w = wkv.next()
                            S.dma("pool", w.t[:], wkvv[:, :, D + hf * 512: D + (hf + 1) * 512], writes=[w])
                            for mt in range(2):
                                ps = PS_A.next()
                                for kc in range(8):
                                    op("pe", lambda t: t.matmul(ps.t[:], lhsT=mT.t[:, kc, mt * 128:(mt + 1) * 128], rhs=w.t[:, kc, :], start=(kc == 0), stop=(kc == 7)), reads=[w, mT], writes=[ps])
                                op("act", lambda a: a.activation(out=Vx.t[:, mt, hf * 512:(hf + 1) * 512], in_=ps.t[:], func=AF.Copy), reads=[ps], writes=[Vx])
                        wqr = Ring([sbt(ed1, "wq%d" % i, [128, 8, 256], BF16) for i in range(2)])
                        wqv = xwq_d.ap().rearrange("(k p) n -> p k n", p=128)
                        qx = sbt(ed1, "qx", [128, 2, 512], BF16)
                        q32x = [sbt(ed1, "q32x%d" % i, [128, 512], F32) for i in range(2)]
                        qsqx = sbt(ed1, "qsqx", [128, 512], BF16)
                        qrs = sbt(ed1, "qrs", [128, 512], F32)
                        pTx = Ring([sbt(ed1, "pTx%d" % i, [128, 512], BF16) for i in range(4)])
                        rdn = sbt(ed1, "rdn", [128, 512], F32)
                        for h in range(4):
                            w = wqr.next()
                            S.dma("pool", w.t[:], wqv[:, :, h * 256:(h + 1) * 256], writes=[w])
                            for tb in range(4):
                                pss = PS_B.next()
                                for dc in range(2):
                                    ps = PS_A.next()
                                    for kc in range(8):
                                        op("pe", lambda t: t.matmul(ps.t[:], lhsT=w.t[:, kc, dc * 128:(dc + 1) * 128], rhs=xT.t[:, kc, tb * 512:(tb + 1) * 512], start=(kc == 0), stop=(kc == 7)), reads=[w, xT], writes=[ps])
                                    op("act", lambda a: a.activation(out=qsqx.t[:], in_=ps.t[:], func=AF.Square), reads=[ps], writes=[qsqx])
                                    op("dve", lambda v: v.tensor_copy(out=q32x[dc].t[:], in_=ps.t[:]), reads=[ps], writes=[q32x[dc]])
                                    op("pe", lambda t: t.matmul(pss.t[:], lhsT=ones_b.t[:], rhs=qsqx.t[:], start=(dc == 0), stop=(dc == 1)), reads=[ones_b, qsqx], writes=[pss])
                                op("act", lambda a: a.activation(out=qrs.t[:], in_=pss.t[:], func=AF.Ln, scale=1.0 / 256, bias=EPS), reads=[pss, vec], writes=[qrs])
                                op("act", lambda a: a.activation(out=qrs.t[:], in_=qrs.t[:], func=AF.Exp, scale=-0.5), reads=[qrs], writes=[qrs])
                                for dc in range(2):
                                    op("dve", lambda v: v.scalar_tensor_tensor(out=qx.t[:, dc, :], in0=q32x[dc].t[:], scalar=V_(81 + dc), in1=qrs.t[:], op0=ALU.mult, op1=ALU.mult), reads=[q32x[dc], qrs, vec], writes=[qx])
                                pts = []
                                for mt in range(2):
                                    ps = PS_A.next()
                                    for dc in range(2):
                                        op("pe", lambda t: t.matmul(ps.t[:], lhsT=kxT.t[:, h * 2 + dc, mt * 128:(mt + 1) * 128], rhs=qx.t[:, dc, :], start=(dc == 0), stop=(dc == 1)), reads=[kxT, qx], writes=[ps])
                                    pT = pTx.next()
                                    op("act", lambda a: a.activation(out=pT.t[:], in_=ps.t[:], func=AF.Exp), reads=[ps], writes=[pT])
                                    pts.append(pT)
                                pd = PS_B.next()
                                for mt in range(2):
                                    op("pe", lambda t: t.matmul(pd.t[:], lhsT=ones_b.t[:], rhs=pts[mt].t[:], start=(mt == 0), stop=(mt == 1)), reads=[ones_b, pts[mt]], writes=[pd])
                                op("act", lambda a: a.activation(out=rdn.t[:], in_=pd.t[:], func=AF.Ln), reads=[pd], writes=[rdn])
                                op("act", lambda a: a.activation(out=rdn.t[:], in_=rdn.t[:], func=AF.Exp, scale=-1.0), reads=[rdn], writes=[rdn])
                                for dc in range(2):
                                    po = PS_B.next()
                                    for mt in range(2):
                                        op("pe", lambda t: t.matmul(po.t[:], lhsT=Vx.t[:, mt, h * 256 + dc * 128: h * 256 + (dc + 1) * 128], rhs=pts[mt].t[:], start=(mt == 0), stop=(mt == 1)), reads=[Vx, pts[mt]], writes=[po])
                                    op("dve", lambda v: v.tensor_tensor(out=OT.t[:, h * 2 + dc, tb * 512:(tb + 1) * 512], in0=po.t[:], in1=rdn.t[:], op=ALU.mult), reads=[po, rdn], writes=[OT])
                        S.barrier("D")
                    with ExitStack() as ed2:
                        wo = sbt(ed2, "wxo", [128, 8, D], BF16)
                        S.dma("pool", wo.t[:], xwo_d.ap().rearrange("(k p) n -> p k n", p=128), writes=[wo])
                        hs = [sbt(ed2, "h2_%d" % i, [128, D], F32) for i in range(2)]
                        junk = sbt(ed2, "junke", [128, D], BF16)
                        xm32s = [sbt(ed2, "xm32_%d" % i, [128, D], F32) for i in range(2)]
                        xm16 = [sbt(ed2, "xm16_%d" % i, [128, D], BF16) for i in range(2)]
                        xmTs = [sbt(ed2, "xmT%d" % i, [128, 8, 128], F32) for i in range(2)]
                        ssx = sbt(ed2, "ssxe", [128, NT], F32)
                        lgs = [sbt(ed2, "lg%d" % i, [128, 36], F32) for i in range(2)]
                        rts = [sbt(ed2, "rt%d" % i, [128, 256], F32) for i in range(3)]
                        ohss = [sbt(ed2, "ohs%d" % i, [128, 32], BF16) for i in range(3)]
                        op("dve", lambda v: v.memset(ssx.t[:], 0.0), writes=[ssx])
                        def stage1(i):
                            ht = hs[i % 2]
                            xm32 = xm32s[i % 2]
                            rows = slice(b * T + i * 128, b * T + (i + 1) * 128)
                            S.dma("sp", ht.t[:], H1_d.ap()[rows, :], writes=[ht])
                            for hf in range(2):
                                ps = PS_A.next()
                                for cc in range(8):
                                    op("pe", lambda t: t.matmul(ps.t[:], lhsT=OT.t[:, cc, i * 128:(i + 1) * 128], rhs=wo.t[:, cc, hf * 512:(hf + 1) * 512], start=(cc == 0), stop=(cc == 7)), reads=[OT, wo], writes=[ps])
                                op("dve", lambda v: v.tensor_tensor(out=ht.t[:, hf * 512:(hf + 1) * 512], in0=ht.t[:, hf * 512:(hf + 1) * 512], in1=ps.t[:], op=ALU.add), reads=[ps, ht], writes=[ht])
                            S.dma("sp", out_d.ap()[rows, :], ht.t[:], reads=[ht], writes=[Buf()])
                            op("act", lambda a: a.activation(out=junk.t[:], in_=ht.t[:], func=AF.Square, accum_out=ssx.t[:, i:i + 1]), reads=[ht], writes=[junk, ssx])
                            rstd_inplace(ssx, ssx.t[:, i:i + 1], D)
                            op("dve", lambda v: v.scalar_tensor_tensor(out=xm32.t[:], in0=ht.t[:], scalar=ssx.t[:, i:i + 1], in1=gmoe_t.t[:], op0=ALU.mult, op1=ALU.mult), reads=[ht, ssx, gmoe_t], writes=[xm32])
                            x16 = xm16[i % 2]
                            op("act", lambda a: a.activation(out=x16.t[:], in_=xm32.t[:], func=AF.Copy), reads=[xm32], writes=[x16])
                            S.dma("sp", XM_d.ap()[rows, :], x16.t[:], reads=[x16], writes=[Buf()])

                        def stage2(i):
                            xm32 = xm32s[i % 2]; xmT = xmTs[i % 2]; lg = lgs[i % 2]; ohs = ohss[i % 3]
                            for half in range(2):
                                ps = PS_C.next()
                                for k4 in range(4):
                                    kc = half * 4 + k4
                                    op("pe", lambda t: t.transpose(out=ps.t[:, k4 * 128:(k4 + 1) * 128], in_=xm32.t[:, kc * 128:(kc + 1) * 128], identity=ident_f.t[:]), reads=[xm32, ident_f], writes=[ps])
                                op("act", lambda a: a.activation(out=xmT.t[:, half * 4:(half + 1) * 4, :], in_=ps.t[:].rearrange("p (k t) -> p k t", k=4), func=AF.Copy), reads=[ps], writes=[xmT])
                            ps = PS_C.next()
                            for kc in range(8):
                                op("pe", lambda t: t.matmul(ps.t[:, 0:36], lhsT=xmT.t[:, kc, :], rhs=wr_t.t[:, kc, :], start=(kc == 0), stop=(kc == 7)), reads=[xmT, wr_t], writes=[ps])
                            op("dve", lambda v: v.tensor_tensor(out=lg.t[:], in0=ps.t[:, 0:36], in1=brc_t.t[:], op=ALU.add), reads=[ps, brc_t], writes=[lg])
                            r = rts[i % 3]
                            R_ = lambda a, b_=None: r.t[:, a:(a + 1 if b_ is None else b_)]
                            op("dve", lambda v: v.tensor_reduce(out=R_(0), in_=lg.t[:, 0:4], axis=AX.X, op=ALU.max), reads=[lg], writes=[r])
                            op("dve", lambda v: v.tensor_scalar(out=R_(1), in0=R_(0), scalar1=-1.0, scalar2=None, op0=ALU.mult), reads=[r], writes=[r])
                            op("dve", lambda v: v.tensor_scalar(out=R_(4, 8), in0=lg.t[:, 0:4], scalar1=R_(0), scalar2=None, op0=ALU.is_equal), reads=[lg, r], writes=[r])
                            op("dve", lambda v: v.memset(R_(2), 0.0), reads=[r], writes=[r])
                            op("act", lambda a: a.activation(out=R_(8, 12), in_=lg.t[:, 0:4], func=AF.Exp, bias=R_(1), accum_out=R_(2)), reads=[lg, r], writes=[r])
                            op("dve", lambda v: v.reciprocal(out=R_(3), in_=R_(2)), reads=[r], writes=[r])
                            op("dve", lambda v: v.tensor_tensor(out=R_(16, 48).rearrange("p (g e) -> p g e", g=4), in0=lg.t[:, 4:36].rearrange("p (g e) -> p g e", g=4),
                                                                in1=R_(4, 8).unsqueeze(2).to_broadcast([128, 4, 8]), op=ALU.mult), reads=[lg, r], writes=[r])
                            op("dve", lambda v: v.tensor_reduce(out=R_(48, 56), in_=R_(16, 48).rearrange("p (g e) -> p e g", g=4), axis=AX.X, op=ALU.add), reads=[r], writes=[r])
                            op("dve", lambda v: v.max(out=R_(56, 64), in_=R_(48, 56)), reads=[r], writes=[r])
                            op("dve", lambda v: v.tensor_tensor(out=R_(64), in0=R_(57), in1=R_(56), op=ALU.subtract), reads=[r], writes=[r])
                            op("act", lambda a: a.activation(out=R_(65), in_=R_(64), func=AF.Exp), reads=[r], writes=[r])
                            op("dve", lambda v: v.tensor_scalar(out=R_(66), in0=R_(65), scalar1=1.0, scalar2=None, op0=ALU.add), reads=[r], writes=[r])
                            op("dve", lambda v: v.reciprocal(out=R_(66), in_=R_(66)), reads=[r], writes=[r])
                            op("dve", lambda v: v.tensor_tensor(out=R_(67), in0=R_(65), in1=R_(66), op=ALU.mult), reads=[r], writes=[r])
                            col = (b * NT + i) * 2
                            op("dve", lambda v: v.tensor_scalar(out=REC.t[:, col:col + 2, 1], in0=R_(66, 68), scalar1=R_(3), scalar2=None, op0=ALU.mult), reads=[r], writes=[REC])
                            op("dve", lambda v: v.tensor_copy(out=REC.t[:, col:col + 2, 0], in_=tokid_t.t[:, b * NT + i: b * NT + i + 1].to_broadcast([128, 2])), reads=[tokid_t], writes=[REC])
                            op("dve", lambda v: v.tensor_scalar(out=R_(68, 76), in0=R_(48, 56), scalar1=R_(56), scalar2=None, op0=ALU.is_equal), reads=[r], writes=[r])
                            op("dve", lambda v: v.tensor_scalar(out=R_(76, 84), in0=R_(48, 56), scalar1=R_(57), scalar2=None, op0=ALU.is_equal), reads=[r], writes=[r])
                            for k in range(2):
                                op("dve", lambda v: v.tensor_tensor(out=R_(96 + 32 * k, 128 + 32 * k).rearrange("p (g e) -> p g e", g=4), in0=R_(4, 8).unsqueeze(2).to_broadcast([128, 4, 8]),
                                                                    in1=R_(68 + 8 * k, 76 + 8 * k).unsqueeze(1).to_broadcast([128, 4, 8]), op=ALU.mult), reads=[r], writes=[r])
                            op("dve", lambda v: v.tensor_tensor(out=ohs.t[:], in0=R_(96, 128), in1=R_(128, 160), op=ALU.add), reads=[r], writes=[ohs])

                        def stage3(i):
                            r = rts[i % 3]; ohs = ohss[i % 3]
                            R_ = lambda a, b_=None: r.t[:, a:(a + 1 if b_ is None else b_)]
                            col = (b * NT + i) * 2
                            ps = PS_C.next()
                            op("pe", lambda t: t.matmul(ps.t[:, 0:32], lhsT=ltri_b.t[:], rhs=ohs.t[:], start=True, stop=True), reads=[ltri_b, ohs], writes=[ps])
                            op("pe", lambda t: t.matmul(ps.t[:, 32:64], lhsT=ones_b.t[:], rhs=ohs.t[:], start=True, stop=True), reads=[ones_b, ohs], writes=[ps])
                            op("dve", lambda v: v.tensor_tensor(out=R_(160, 192), in0=ps.t[:, 0:32], in1=carry.t[:], op=ALU.add), reads=[ps, carry], writes=[r])
                            op("dve", lambda v: v.tensor_tensor(out=carry.t[:], in0=carry.t[:], in1=ps.t[:, 32:64], op=ALU.add), reads=[ps, carry], writes=[carry])
                            for k in range(2):
                                op("dve", lambda v: v.tensor_tensor(out=R_(192, 224), in0=R_(96 + 32 * k, 128 + 32 * k), in1=R_(160, 192), op=ALU.mult), reads=[r], writes=[r])
                                op("dve", lambda v: v.tensor_reduce(out=POSA.t[:, col + k:col + k + 1], in_=R_(192, 224), axis=AX.X, op=ALU.add), reads=[r], writes=[POSA])
                                op("dve", lambda v: v.tensor_tensor(out=R_(192, 224), in0=R_(96 + 32 * k, 128 + 32 * k), in1=ec_t.t[:], op=ALU.mult), reads=[r, ec_t], writes=[r])
                                op("dve", lambda v: v.tensor_reduce(out=EXPA.t[:, col + k:col + k + 1], in_=R_(192, 224), axis=AX.X, op=ALU.add), reads=[r], writes=[EXPA])

                        for i in range(NT + 2):
                            if i < NT:
                                stage1(i)
                            if 1 <= i <= NT:
                                stage2(i - 1)
                            if i >= 2:
                                stage3(i - 2)
                        S.barrier("E")
        if stop_after in ("A", "B", "C", "D"):
            S.finish("sp")
            return nc, ins, dbg_out

        NC2 = NB * NT * 2
        with ExitStack() as em:
            pt_ = sbt(em, "pt_", [128, 8, 32], F32)
            pti = sbt(em, "pti", [128, 32], I32)
            op("dve", lambda v: v.tensor_scalar(out=pt_.t[:, 0, :], in0=carry.t[:], scalar1=float(BS - 1), scalar2=None, op0=ALU.add), reads=[carry], writes=[pt_])
            op("dve", lambda v: v.tensor_copy(out=pti.t[:], in_=pt_.t[:, 0, :]), reads=[pt_], writes=[pti])
            op("dve", lambda v: v.tensor_single_scalar(out=pti.t[:], in_=pti.t[:], scalar=BS_SH, op=ALU.arith_shift_right), reads=[pti], writes=[pti])
            op("dve", lambda v: v.tensor_single_scalar(out=pti.t[:], in_=pti.t[:], scalar=BS_SH, op=ALU.logical_shift_left), reads=[pti], writes=[pti])
            op("dve", lambda v: v.tensor_copy(out=pt_.t[:, 1, :], in_=pti.t[:]), reads=[pti], writes=[pt_])
            op("dve", lambda v: v.memset(pt_.t[:, 4, :], 1.0), reads=[pt_], writes=[pt_])
            op("dve", lambda v: v.tensor_tensor_scan(out=pt_.t[:, 2, :], data0=pt_.t[:, 4, :], data1=pt_.t[:, 1, :], initial=0.0, op0=ALU.mult, op1=ALU.add), reads=[pt_], writes=[pt_])
            op("dve", lambda v: v.tensor_tensor(out=pt_.t[:, 3, :], in0=pt_.t[:, 2, :], in1=pt_.t[:, 1, :], op=ALU.subtract), reads=[pt_], writes=[pt_])
            cmpb = sbt(em, "cmpb", [128, NBLK, 32], F32)
            bexp = sbt(em, "bexp", [128, NBLK], F32)
            widx = sbt(em, "widx", [128, NBLK], I32)
            op("dve", lambda v: v.tensor_tensor(out=cmpb.t[:], in0=pt_.t[:, 2, :].unsqueeze(1).to_broadcast([128, NBLK, 32]), in1=blk0_t.t[:].unsqueeze(2).to_broadcast([128, NBLK, 32]), op=ALU.is_le), reads=[pt_, blk0_t], writes=[cmpb])
            op("dve", lambda v: v.tensor_reduce(out=bexp.t[:], in_=cmpb.t[:], axis=AX.X, op=ALU.add), reads=[cmpb], writes=[bexp])
            op("dve", lambda v: v.tensor_scalar(out=bexp.t[:], in0=bexp.t[:], scalar1=31.0, scalar2=128.0, op0=ALU.min, op1=ALU.mult), reads=[bexp], writes=[bexp])
            op("dve", lambda v: v.tensor_scalar(out=bexp.t[:], in0=bexp.t[:], scalar1=tokid_t.t[:, 0:1], scalar2=None, op0=ALU.add), reads=[bexp, tokid_t], writes=[bexp])
            op("dve", lambda v: v.tensor_copy(out=widx.t[:], in_=bexp.t[:]), reads=[bexp], writes=[widx])
            with ExitStack() as em0:
                oha = sbt(em0, "oha", [128, NC2, 32], F32)
                dst = sbt(em0, "dst", [128, NC2], F32)
                dsti = sbt(em0, "dsti", [128, NC2], I32)
                op("dve", lambda v: v.tensor_tensor(out=oha.t[:], in0=EXPA.t[:].unsqueeze(2).to_broadcast([128, NC2, 32]), in1=ec_t.t[:].unsqueeze(1).to_broadcast([128, NC2, 32]), op=ALU.is_equal), reads=[EXPA, ec_t], writes=[oha])
                op("dve", lambda v: v.tensor_tensor(out=oha.t[:], in0=oha.t[:], in1=pt_.t[:, 3, :].unsqueeze(1).to_broadcast([128, NC2, 32]), op=ALU.mult), reads=[oha, pt_], writes=[oha])
                op("dve", lambda v: v.tensor_reduce(out=dst.t[:], in_=oha.t[:], axis=AX.X, op=ALU.add), reads=[oha], writes=[dst])
                op("dve", lambda v: v.tensor_tensor(out=dst.t[:], in0=dst.t[:], in1=POSA.t[:], op=ALU.add), reads=[dst, POSA], writes=[dst])
                op("dve", lambda v: v.tensor_copy(out=dsti.t[:], in_=dst.t[:]), reads=[dst], writes=[dsti])
                for c_ in range(NC2):
                    S.scatter(SLOT_d.ap()[:, :], REC.t[:, c_, :], dsti.t[:, c_:c_ + 1], reads=[REC, dsti, SLb], writes=[Buf()])
                S.barrier("slots")
            if "moe" in dbg:
                S.dma("sp", ddbg("pt", [128, 8 * 32]).ap()[:, :], pt_.t[:].rearrange("p a b -> p (a b)"), reads=[pt_])
                S.dma("sp", ddbg("widx", [128, NBLK], I32).ap()[:, :], widx.t[:], reads=[widx])
            w1r = Ring([sbt(em, "ew1_%d" % i, [128, 8, 512], BF16) for i in range(3)])
            w3r = Ring([sbt(em, "ew3_%d" % i, [128, 8, 512], BF16) for i in range(3)])
            w2r = Ring([sbt(em, "ew2_%d" % i, [128, 4, D], BF16) for i in range(3)])
            NJ = BS // 128
            recs = Ring([sbt(em, "mrec%d" % i, [128, NJ, 2], F32) for i in range(3)])
            idxs = Ring([sbt(em, "midx%d" % i, [128, NJ], I32) for i in range(3)])
            xg = Ring([sbt(em, "xg%d" % i, [128, D], BF16) for i in range(3 * (BS // 128))])
            xgT = Ring([sbt(em, "xgT%d" % i, [128, 8, BS], BF16) for i in range(2)])
            GT = Ring([sbt(em, "GT%d" % i, [128, 4, BS], BF16) for i in range(2)])
            s1 = Ring([sbt(em, "s1_%d" % i, [128, BS], F32) for i in range(2)])
            yw = Ring([sbt(em, "yw%d" % i, [128, D], F32) for i in range(4)])
            ew1v = EWB_d[0].ap()[:, :]
            ew3v = EWB_d[1].ap()[:, :]
            ew2v = EWB_d[2].ap()[:, :]
            state = {}

            def prepA(blk):
                w1 = w1r.next(); w3 = w3r.next(); w2 = w2r.next()
                S.gather(w1.t[:].rearrange("p k n -> p (k n)"), ew1v, widx.t[:, blk:blk + 1], reads=[widx], writes=[w1])
                S.gather(w3.t[:].rearrange("p k n -> p (k n)"), ew3v, widx.t[:, blk:blk + 1], reads=[widx], writes=[w3])
                S.gather(w2.t[:].rearrange("p k n -> p (k n)"), ew2v, widx.t[:, blk:blk + 1], reads=[widx], writes=[w2])
                rc = recs.next(); ix = idxs.next()
                S.dma("sp", rc.t[:], SLOT_d.ap()[blk * BS:(blk + 1) * BS, :].rearrange("(p j) c -> p j c", p=128), writes=[rc])
                op("dve", lambda v: v.tensor_copy(out=ix.t[:], in_=rc.t[:, :, 0]), reads=[rc], writes=[ix])
                gs = []
                for j in range(NJ):
                    g_ = xg.next()
                    S.gather(g_.t[:], XM_d.ap()[:, :], ix.t[:, j:j + 1], reads=[ix], writes=[g_])
                    gs.append(g_)
                state[blk] = dict(w1=w1, w3=w3, w2=w2, rc=rc, ix=ix, gs=gs)

            def prepB(blk):
                st_ = state[blk]
                xt_ = xgT.next()
                for j in range(NJ):
                    g_ = st_["gs"][j]
                    ps = PS_C.next()
                    psv = ps.t[:].bitcast(BF16)
                    for kc in range(8):
                        op("pe", lambda t: t.transpose(out=psv[:, kc * 128:(kc + 1) * 128], in_=g_.t[:, kc:D:8], identity=ident_b.t[:]), reads=[g_, ident_b], writes=[ps])
                    op("act", lambda a: a.activation(out=xt_.t[:, :, j * 128:(j + 1) * 128], in_=psv.rearrange("p (k t) -> p k t", k=8), func=AF.Copy), reads=[ps], writes=[xt_])
                st_["xt"] = xt_

            def computeH(blk):
                st_ = state[blk]
                w1, w3, xt_ = st_["w1"], st_["w3"], st_["xt"]
                gt = GT.next()
                st_["gt"] = gt
                for fc in range(4):
                    p1 = PS_A.next()
                    for kc in range(8):
                        op("pe", lambda t: t.matmul(p1.t[:, 0:BS], lhsT=w1.t[:, kc, fc:512:4], rhs=xt_.t[:, kc, :], start=(kc == 0), stop=(kc == 7)), reads=[w1, xt_], writes=[p1])
                    p3 = PS_B.next()
                    for kc in range(8):
                        op("pe", lambda t: t.matmul(p3.t[:, 0:BS], lhsT=w3.t[:, kc, fc:512:4], rhs=xt_.t[:, kc, :], start=(kc == 0), stop=(kc == 7)), reads=[w3, xt_], writes=[p3])
                    s_ = s1.next()
                    op("act", lambda a: a.activation(out=s_.t[:], in_=p1.t[:, 0:BS], func=AF.Silu), reads=[p1], writes=[s_])
                    op("dve", lambda v: v.tensor_tensor(out=gt.t[:, fc, :], in0=s_.t[:], in1=p3.t[:, 0:BS], op=ALU.mult), reads=[s_, p3], writes=[gt])

            def computeY(blk):
                st_ = state.pop(blk)
                w2, rc, ix, gt = st_["w2"], st_["rc"], st_["ix"], st_["gt"]
                for ev_ in prev_sc:
                    S._wait("pool", ev_)
                del prev_sc[:]
                for j in range(NJ):
                    y_ = yw.next()
                    for hf in range(2):
                        ps = PS_A.next()
                        for fc in range(4):
                            op("pe", lambda t: t.matmul(ps.t[:], lhsT=gt.t[:, fc, j * 128:(j + 1) * 128], rhs=w2.t[:, fc, hf * 512:(hf + 1) * 512], start=(fc == 0), stop=(fc == 3)), reads=[gt, w2], writes=[ps])
                        if hf == 0:
                            op("act", lambda a: a.activation(out=y_.t[:, hf * 512:(hf + 1) * 512], in_=ps.t[:], func=AF.Copy, scale=rc.t[:, j, 1:2]), reads=[ps, rc], writes=[y_])
                        else:
                            op("dve", lambda v: v.tensor_scalar(out=y_.t[:, hf * 512:(hf + 1) * 512], in0=ps.t[:], scalar1=rc.t[:, j, 1:2], scalar2=None, op0=ALU.mult), reads=[ps, rc], writes=[y_])
                    prev_sc.append(S.scatter(out_d.ap()[:, :], y_.t[:], ix.t[:, j:j + 1], reads=[y_, ix], writes=[Buf()], add=True))

            prev_sc = []
            prepA(0)
            if NBLK > 1:
                prepA(1)
            prepB(0)
            for blk in range(NBLK):
                if blk + 2 < NBLK:
                    prepA(blk + 2)
                computeH(blk)
                if blk + 1 < NBLK:
                    prepB(blk + 1)
                computeY(blk)
            S.finish("sp")
    return nc, ins, dbg_out


def _core_inputs(inp, NB, b0, cst=None):
    f = lambda a: np.ascontiguousarray(np.asarray(a, np.float32))
    m = {}
    m["x"] = f(inp["x"][b0:b0 + NB]).reshape(NB * T, D)
    m["mem"] = f(inp["mem"][b0:b0 + NB]).reshape(NB * ML, D)
    return m


def _shared_inputs(inp, NB):
    f = lambda a: np.ascontiguousarray(np.asarray(a, np.float32))
    m = {}
    m["rel_bias"] = f(inp["rel_bias"])
    m["w_in"] = f(inp["w_in"][0])
    m["vecs"] = _pack_vecs(inp)
    m["gmoe_b"] = np.ascontiguousarray(np.broadcast_to(f(inp["norm_moe"][0])[None], (128, D)))
    brc = np.concatenate([f(inp["router_g_b"][0]), f(inp["router_e_b"][0])])
    m["br_b"] = np.ascontiguousarray(np.broadcast_to(brc[None], (128, 36)))
    m["wbd_r"] = _bd(inp["rg_w_r"][0]); m["wbd_i"] = _bd(inp["rg_w_i"][0])
    m["posT_k"] = f(np.asarray(inp["cmp_pos_k"][0]).T); m["posT_v"] = f(np.asarray(inp["cmp_pos_v"][0]).T)
    m["ck_w1"] = f(inp["cmp_k_w1"][0]); m["ck_w2"] = f(inp["cmp_k_w2"][0])
    m["cv_w1"] = f(inp["cmp_v_w1"][0]); m["cv_w2"] = f(inp["cmp_v_w2"][0])
    m["w_out"] = f(inp["w_out"][0]); m["xa_wq"] = f(inp["xa_w_q"][0]); m["xa_wkv"] = f(inp["xa_w_kv"][0]); m["xa_wo"] = f(inp["xa_w_o"][0])
    m["wr_cat"] = np.ascontiguousarray(np.concatenate([f(inp["router_g_w"][0]), f(inp["router_e_w"][0])], axis=1))
    m["exp_w1"] = f(inp["exp_w1"][0]); m["exp_w3"] = f(inp["exp_w3"][0]); m["exp_w2"] = f(inp["exp_w2"][0])
    m.update(_static_consts(NB))
    return m


def kernel(**inputs):
    NB = 32 // N_CORES
    nc, ins, _ = build(NB)
    shared = _shared_inputs(inputs, NB)
    in_maps = []
    for c in range(N_CORES):
        m = dict(shared)
        m.update(_core_inputs(inputs, NB, c * NB))
        in_maps.append(m)
    res = run_bass_kernel_spmd(nc, in_maps, core_ids=list(range(N_CORES)))
    outs = [np.asarray(r["out"])[:NB * T].reshape(NB, T, D) for r in res.results]
    return np.concatenate(outs, axis=0).astype(np.float32)
```

```python
import math
from contextlib import ExitStack
import numpy as np
import concourse.bass as bass
import concourse.mybir as mybir
from concourse.bass_utils import run_bass_kernel_spmd

F32 = mybir.dt.float32
BF16 = mybir.dt.bfloat16
I32 = mybir.dt.int32
AF = mybir.ActivationFunctionType
ALU = mybir.AluOpType
AX = mybir.AxisListType

T = 2048
D = 1024
NT = 16
PC = 2328
ML = 256
NEXP = 32
BS = 256
BS_SH = 8
NEG = -30000.0
LW, LS, LC = 768, 512, 4096
KF = LW + LS + LC
N_CORES = 8


class Buf:
    __slots__ = ("w", "r", "name", "x")

    def __init__(self, name="", x=False):
        self.w = None
        self.r = {}
        self.name = name
        self.x = x


class TB:
    def __init__(self, t, name=""):
        self.t = t
        self.b = Buf(name)


def _bufs(xs):
    return [x.b if isinstance(x, TB) else x for x in xs]


class Sched:
    def __init__(self, nc, es, n_dma_sems=48):
        self.nc = nc
        self.eng = {"pe": nc.tensor, "act": nc.scalar, "dve": nc.vector, "pool": nc.gpsimd, "sp": nc.sync}
        self.sem = {}
        self.cnt = {}
        self.seen = {e: {} for e in self.eng}
        for e in self.eng:
            self.sem[e] = es.enter_context(nc.semaphore("s_" + e))
            self.cnt[e] = 0
        self.dsem = [es.enter_context(nc.semaphore("d%d" % i)) for i in range(n_dma_sems)]
        self.dcnt = [0] * n_dma_sems
        self.dq = [0, 0]

    def _semobj(self, key):
        return self.sem[key] if isinstance(key, str) else self.dsem[key]

    def _wait(self, e, ev):
        if ev is None:
            return
        key, val = ev
        if self.seen[e].get(key, 0) >= val:
            return
        self.eng[e].wait_ge(self._semobj(key), val)
        self.seen[e][key] = val

    def _deps(self, e, reads, writes, xreads=()):
        for b in list(reads) + list(xreads):
            if b.w is not None and not (e == "pe" and b.w[0] == "pe"):
                self._wait(e, b.w)
        for b in list(writes) + list(xreads):
            if b.w is not None and not (e == "pe" and b.w[0] == "pe"):
                self._wait(e, b.w)
            for ev in list(b.r.items()):
                if not (e == "pe" and ev[0] == "pe"):
                    self._wait(e, ev)

    def _mark(self, ev, reads, writes):
        key, val = ev
        for b in reads:
            b.r[key] = max(b.r.get(key, 0), val)
        for b in writes:
            b.w = ev
            b.r = {}

    def op(self, e, fn, reads=(), writes=(), force=()):
        reads = _bufs(reads)
        writes = _bufs(writes)
        xreads = [b for b in reads if b.x]
        reads = [b for b in reads if not b.x]
        self._deps(e, reads, writes, xreads)
        for ev_ in force:
            self._wait(e, ev_)
        ins = fn(self.eng[e])
        self.cnt[e] += 1
        ins.then_inc(self.sem[e], 1)
        ev = (e, self.cnt[e])
        self._mark(ev, reads, writes + xreads)
        return ev

    def _dma_common(self, e, reads, writes, issue):
        reads = _bufs(reads)
        writes = _bufs(writes)
        self._deps(e, reads, writes)
        half = len(self.dsem) // 2
        q = 0 if e == "sp" else 1
        i = q * half + self.dq[q]
        self.dq[q] = (self.dq[q] + 1) % half
        if self.dcnt[i] > 0:
            self._wait(e, (i, self.dcnt[i]))
        self.dcnt[i] += 16
        issue().then_inc(self.dsem[i], 16)
        ev = (i, self.dcnt[i])
        self._mark(ev, reads, writes)
        return ev

    def dma(self, e, out, in_, reads=(), writes=(), **kw):
        return self._dma_common(e, reads, writes, lambda: self.eng[e].dma_start(out=out, in_=in_, **kw))

    def gather(self, out, src, idx_ap, reads=(), writes=()):
        return self._dma_common("pool", reads, writes, lambda: self.nc.gpsimd.indirect_dma_start(
            out=out, out_offset=None, in_=src, in_offset=bass.IndirectOffsetOnAxis(ap=idx_ap, axis=0)))

    def scatter(self, dst, src, idx_ap, reads=(), writes=(), add=False):
        kw = {"compute_op": ALU.add} if add else {}
        return self._dma_common("pool", reads, writes, lambda: self.nc.gpsimd.indirect_dma_start(
            out=dst, out_offset=bass.IndirectOffsetOnAxis(ap=idx_ap, axis=0), in_=src, in_offset=None, **kw))

    def barrier(self, label=None):
        if label is not None:
            MARKS.append((label, dict(self.cnt)))
        for e in self.eng:
            for e2 in self.eng:
                if e2 != e and self.cnt[e2] > 0:
                    self._wait(e, (e2, self.cnt[e2]))
            for i in range(len(self.dsem)):
                if self.dcnt[i] > 0:
                    self._wait(e, (i, self.dcnt[i]))

    def finish(self, e="sp"):
        for e2 in self.eng:
            if e2 != e and self.cnt[e2] > 0:
                self._wait(e, (e2, self.cnt[e2]))
        for i in range(len(self.dsem)):
            if self.dcnt[i] > 0:
                self._wait(e, (i, self.dcnt[i]))


MARKS = []


class _Stop(Exception):
    pass


class Ring:
    def __init__(self, items):
        self.items = items
        self.i = 0

    def next(self):
        x = self.items[self.i]
        self.i = (self.i + 1) % len(self.items)
        return x


def _bucket(n):
    n = np.maximum(n, 0)
    nf = np.maximum(n, 1).astype(np.float32)
    large = 16 + (np.log(nf / np.float32(16)) / np.float32(math.log(128 / 16)) * np.float32(16)).astype(np.int32)
    large = np.minimum(large, 31)
    return np.where(n < 16, n, large)


def _static_consts(NB):
    c = {}
    c["c_ident"] = np.eye(128, dtype=np.float32)
    oh = np.zeros((33, KF), np.float32)
    k = np.arange(LW); dist = k - 128
    valid = (dist >= 0) & (dist < 512)
    oh[_bucket(dist)[valid], k[valid]] = 1.0; oh[32, k[~valid]] = 1.0
    k = np.arange(LS); dist = k - 128
    valid = dist >= 0
    oh[_bucket(dist)[valid], LW + k[valid]] = 1.0; oh[32, LW + k[~valid]] = 1.0
    k = np.arange(LC); dist = k - 2048
    valid = dist >= 15
    oh[_bucket(dist)[valid], LW + LS + k[valid]] = 1.0; oh[32, LW + LS + k[~valid]] = 1.0
    c["c_oh"] = oh
    E = np.zeros((32, T), np.float32)
    E[np.arange(T) // 64, np.arange(T)] = 1.0
    c["c_E"] = E
    cs = np.arange(128) * 16; ce = cs + 31
    ss = np.arange(32) * 64
    ov = ((cs[:, None] <= ss[None, :] + 63) & (ce[:, None] >= ss[None, :])).astype(np.float32)
    ov[127] = 0.0
    c["c_ov"] = ov
    t = np.arange(T); cur = t // 64; j = np.arange(32)[None, :]
    forced = (j == 0) | (j == cur[:, None]) | (j == cur[:, None] - 1)
    future = j > cur[:, None]
    keep = (~(forced | future)).astype(np.float32)
    addm = np.where(forced, 1e9, np.where(future, -1e9, 0.0)).astype(np.float32)
    c["c_keep"] = np.ascontiguousarray(keep.reshape(NT, 128, 32).transpose(1, 0, 2).reshape(128, NT * 32))
    c["c_addm"] = np.ascontiguousarray(addm.reshape(NT, 128, 32).transpose(1, 0, 2).reshape(128, NT * 32))
    a = np.arange(128)
    c["c_ltri"] = (a[:, None] < a[None, :]).astype(np.float32)
    c["c_ec"] = np.broadcast_to(np.arange(32).astype(np.float32)[None], (128, 32)).copy()
    nblk = NB * T * 2 // BS + NEXP
    c["c_blk0"] = np.broadcast_to((np.arange(nblk) * BS).astype(np.float32)[None], (128, nblk)).copy()
    c["c_tokid"] = (np.arange(NB * NT)[None, :] * 128 + a[:, None]).astype(np.float32)
    nslot = nblk * BS
    si = np.zeros((nslot, 2), np.float32)
    si[:, 0] = NB * T + (np.arange(nslot) % BS) // (BS // 128)
    c["c_slotinit"] = si.reshape(128, -1)
    return c


def _pcol(v, k):
    return np.ascontiguousarray(np.asarray(v, np.float32).reshape(k, 128).T)


NV = 96


def _pack_vecs(inp):
    v = np.zeros((128, NV), np.float32)
    v[:, 0:8] = _pcol(inp["norm_mix"][0], 8)
    v[:, 8:16] = _pcol(inp["norm_x"][0], 8)
    v[:, 16:24] = _pcol(inp["norm_mem"][0], 8)
    v[:, 24:28] = _pcol(inp["out_g_rg"][0], 4)
    v[:, 28:32] = _pcol(inp["out_g_nsa"][0], 4)
    cw = np.asarray(inp["rg_conv_w"][0]).reshape(4, 512)
    for c in range(4):
        for j in range(4):
            v[:, 32 + c * 4 + j] = cw[j, c * 128:(c + 1) * 128]
    v[:, 48:52] = _pcol(inp["rg_conv_b"][0], 4)
    v[:, 52:56] = _pcol(inp["rg_b_r"][0], 4)
    v[:, 56:60] = _pcol(inp["rg_b_i"][0], 4)
    v[:, 60:64] = _pcol(inp["rg_lambda"][0], 4)
    v[:, 64] = np.tile(np.asarray(inp["nsa_g_q"][0]), 2)
    v[:, 65] = np.tile(np.asarray(inp["nsa_g_ks"][0]), 2)
    v[:, 66] = np.tile(np.asarray(inp["nsa_g_kw"][0]), 2)
    v[:, 67] = np.tile(np.asarray(inp["nsa_g_kc"][0]), 2)
    v[:, 68:70] = _pcol(inp["xa_g_q"][0], 2)
    v[:, 70:72] = _pcol(inp["xa_g_k"][0], 2)
    return v


def _bd(w):
    w = np.asarray(w, np.float32)
    o = np.zeros((128, 4, 128), np.float32)
    for c in range(4):
        o[0:64, c, 0:64] = w[2 * c]
        o[64:128, c, 64:128] = w[2 * c + 1]
    return o


def build(NB=4, dbg=(), stop_after=None):
    nc = bass.Bass("TRN2", target_bir_lowering=False)
    NTOK = NB * T
    NBLK = NTOK * 2 // BS + NEXP
    NSLOT = NBLK * BS
    ins = {}

    def din(name, shape, dt=F32):
        ins[name] = nc.dram_tensor(name, list(shape), dt, kind="ExternalInput")
        return ins[name]

    x_d = din("x", [NTOK, D]); mem_d = din("mem", [NB * ML, D])
    relb_d = din("rel_bias", [32, 8]); win_d = din("w_in", [D, PC])
    vecs_d = din("vecs", [128, NV]); gmoe_d = din("gmoe_b", [128, D]); brc_d = din("br_b", [128, 36])
    wbdr_d = din("wbd_r", [128, 4, 128]); wbdi_d = din("wbd_i", [128, 4, 128])
    posk_d = din("posT_k", [64, 32]); posv_d = din("posT_v", [64, 32])
    ckw1_d = din("ck_w1", [2048, 256]); ckw2_d = din("ck_w2", [256, 64])
    cvw1_d = din("cv_w1", [2048, 256]); cvw2_d = din("cv_w2", [256, 64])
    wout_d = din("w_out", [D, D]); xwq_d = din("xa_wq", [D, D]); xwkv_d = din("xa_wkv", [D, 2 * D]); xwo_d = din("xa_wo", [D, D])
    wr_d = din("wr_cat", [D, 36])
    ew1_d = din("exp_w1", [NEXP, D, 512]); ew3_d = din("exp_w3", [NEXP, D, 512]); ew2_d = din("exp_w2", [NEXP, 512, D])
    cst = _static_consts(NB)
    cd = {k: din(k, v.shape) for k, v in cst.items()}
    out_d = nc.dram_tensor("out", [NTOK + 128, D], F32, kind="ExternalOutput")
    FW_d = nc.dram_tensor("FW", [8, 128, LW], F32, kind="Internal")
    FS_d = nc.dram_tensor("FS", [8, 128, LS], F32, kind="Internal")
    FC_d = nc.dram_tensor("FC", [8, 128, LC], F32, kind="Internal")
    CB_d = nc.dram_tensor("CB", [2 * NT, 128, 512], BF16, kind="Internal")
    H1_d = nc.dram_tensor("H1", [NTOK, D], F32, kind="Internal")
    XM_d = nc.dram_tensor("XM", [NTOK + 128, D], BF16, kind="Internal")
    SLOT_d = nc.dram_tensor("SLOT", [NSLOT, 2], F32, kind="Internal")
    EWB_d = [nc.dram_tensor("EWB%d" % i, [NEXP * 128, 4096], BF16, kind="Internal") for i in range(3)]
    dbg_out = {}

    def ddbg(name, shape, dt=F32):
        dbg_out[name] = nc.dram_tensor("dbg_" + name, list(shape), dt, kind="ExternalOutput")
        return dbg_out[name]

    with ExitStack() as es:
        S = Sched(nc, es)
        op = S.op

        def _on_exit(et, ev, tb_):
            if et is _Stop:
                S.finish("sp")
                return True
            return False

        es.push(_on_exit)

        def stop_here(tag):
            if stop_after == tag:
                S.barrier()
                raise _Stop()

        uid = [0]

        def sbt(stack, name, shape, dt):
            uid[0] += 1
            return TB(stack.enter_context(nc.sbuf_tensor("%s_%d" % (name, uid[0]), list(shape), dt)), name)

        psb = [TB(es.enter_context(nc.psum_tensor("ps%d" % i, [128, 512], F32)), "ps%d" % i) for i in range(8)]
        for p_ in psb:
            p_.b.x = True
        PS_A = Ring(psb[0:3])
        PS_B = Ring(psb[3:6])
        PS_C = Ring(psb[6:8])

        ident_f = sbt(es, "ident_f", [128, 128], F32)
        ident_b = sbt(es, "ident_b", [128, 128], BF16)
        ones_b = sbt(es, "ones_b", [128, 128], BF16)
        onesbd_b = sbt(es, "onesbd_b", [128, 128], BF16)
        ltri_b = sbt(es, "ltri_b", [128, 128], BF16)
        E_b = sbt(es, "E_b", [32, T], BF16)
        ov_b = sbt(es, "ov_b", [128, 32], BF16)
        keep_t = sbt(es, "keep_t", [128, NT, 32], F32)
        addm_t = sbt(es, "addm_t", [128, NT, 32], F32)
        ec_t = sbt(es, "ec_t", [128, 32], F32)
        tokid_t = sbt(es, "tokid_t", [128, NB * NT], F32)
        vec = sbt(es, "vec", [128, NV], F32)
        gmoe_t = sbt(es, "gmoe_t", [128, D], F32)
        brc_t = sbt(es, "brc_t", [128, 36], F32)
        wbdr = sbt(es, "wbdr", [128, 4, 128], BF16)
        wbdi = sbt(es, "wbdi", [128, 4, 128], BF16)
        cbk = sbt(es, "cbk", [128, 4], F32)
        WB = sbt(es, "WB", [128, 2, 5, 512], BF16)
        SB = sbt(es, "SB", [128, 2, 3, 512], BF16)
        wr_t = sbt(es, "wr_t", [128, 8, 36], F32)
        carry = sbt(es, "carry", [128, 32], F32)
        blk0_t = sbt(es, "blk0_t", [128, NBLK], F32)
        REC = sbt(es, "REC", [128, NB * NT * 2, 2], F32)
        POSA = sbt(es, "POSA", [128, NB * NT * 2], F32)
        EXPA = sbt(es, "EXPA", [128, NB * NT * 2], F32)
        V_ = lambda a, b=None: vec.t[:, a:(a + 1 if b is None else b)]

        ld = lambda tb, src: S.dma("sp", tb.t[:], src, writes=[tb])
        ldc = lambda tb, src: S.dma("pool", tb.t[:], src, writes=[tb])
        ld(ident_f, cd["c_ident"].ap()[:, :]); ldc(ident_b, cd["c_ident"].ap()[:, :])
        ldc(ltri_b, cd["c_ltri"].ap()[:, :]); ldc(E_b, cd["c_E"].ap()[:, :]); ldc(ov_b, cd["c_ov"].ap()[:, :])
        S.dma("sp", keep_t.t[:].rearrange("p a b -> p (a b)"), cd["c_keep"].ap()[:, :], writes=[keep_t])
        S.dma("sp", addm_t.t[:].rearrange("p a b -> p (a b)"), cd["c_addm"].ap()[:, :], writes=[addm_t])
        ld(ec_t, cd["c_ec"].ap()[:, :]); ld(tokid_t, cd["c_tokid"].ap()[:, :]); ld(blk0_t, cd["c_blk0"].ap()[:, :])
        ld(vec, vecs_d.ap()[:, :]); ld(gmoe_t, gmoe_d.ap()[:, :]); ld(brc_t, brc_d.ap()[:, :])
        ldc(wbdr, wbdr_d.ap()[:, :, :]); ldc(wbdi, wbdi_d.ap()[:, :, :])
        S.dma("sp", wr_t.t[:], wr_d.ap().rearrange("(k p) n -> p k n", p=128), writes=[wr_t])
        op("pool", lambda p: p.memset(ones_b.t[:], 1.0), writes=[ones_b])
        op("pool", lambda p: p.memset(onesbd_b.t[:], 0.0), writes=[onesbd_b])
        op("pool", lambda p: p.memset(onesbd_b.t[0:64, 0:64], 1.0), writes=[onesbd_b])
        op("pool", lambda p: p.memset(onesbd_b.t[64:128, 64:128], 1.0), writes=[onesbd_b])
        op("pool", lambda p: p.memset(carry.t[:], 0.0), writes=[carry])
        op("dve", lambda v: v.memset(V_(84), 1e-6), reads=[vec], writes=[vec])
        op("act", lambda a: a.activation(out=V_(72, 76), in_=V_(60, 64), func=AF.Exp, scale=-1.0), reads=[vec], writes=[vec])
        op("act", lambda a: a.activation(out=V_(72, 76), in_=V_(72, 76), func=AF.Ln, bias=1.0), reads=[vec], writes=[vec])
        op("dve", lambda v: v.tensor_scalar(out=V_(76, 80), in0=V_(72, 76), scalar1=-16.0, scalar2=None, op0=ALU.mult), reads=[vec], writes=[vec])
        op("dve", lambda v: v.tensor_scalar(out=V_(72, 76), in0=V_(72, 76), scalar1=-8.0, scalar2=None, op0=ALU.mult), reads=[vec], writes=[vec])
        op("dve", lambda v: v.tensor_scalar(out=V_(80), in0=V_(64), scalar1=0.125, scalar2=None, op0=ALU.mult), reads=[vec], writes=[vec])
        op("dve", lambda v: v.tensor_scalar(out=V_(81, 83), in0=V_(68, 70), scalar1=1.0 / 16.0, scalar2=None, op0=ALU.mult), reads=[vec], writes=[vec])
        EPS = V_(84)

        stop_here("s0")
        with ExitStack() as st:
            zt = sbt(st, "zt", [128, 2 * D], BF16)
            op("pool", lambda p: p.memset(zt.t[:], 0.0), writes=[zt])
            XMb = Buf("XM"); SLb = Buf("SLOT"); OUTb = Buf("OUT")
            S.dma("sp", XM_d.ap()[NTOK:NTOK + 128, :], zt.t[:, 0:D], reads=[zt], writes=[XMb])
            S.dma("sp", out_d.ap()[NTOK:NTOK + 128, :], zt.t[:].bitcast(F32), reads=[zt], writes=[OUTb])
            sl0 = sbt(st, "sl0", [128, NSLOT // 128 * 2], F32)
            ld(sl0, cd["c_slotinit"].ap()[:, :])
            S.dma("sp", SLOT_d.ap().rearrange("(p a) c -> p (a c)", p=128), sl0.t[:], reads=[sl0], writes=[SLb])
            stop_here("s1")
            tab = sbt(st, "tab", [33, 8], F32)
            op("pool", lambda p: p.memset(tab.t[:], NEG), writes=[tab])
            S.dma("sp", tab.t[0:32, :], relb_d.ap()[:, :], writes=[tab])
            tabrep = sbt(st, "tabrep", [33, 8, 128], F32)
            op("dve", lambda v: v.tensor_copy(out=tabrep.t[:], in_=tab.t[:].unsqueeze(2).to_broadcast([33, 8, 128])), reads=[tab], writes=[tabrep])
            oh = sbt(st, "oh", [33, KF], F32)
            ld(oh, cd["c_oh"].ap()[:, :])
            frow = [sbt(st, "frow%d" % i, [128, KF], F32) for i in range(2)]
            Fb = Buf("F")
            for h in range(8):
                fr = frow[h % 2]
                c0 = 0
                while c0 < KF:
                    n = min(512, KF - c0)
                    ps = PS_A.next()
                    op("pe", lambda t: t.matmul(ps.t[:, 0:n], lhsT=tabrep.t[:, h, :], rhs=oh.t[:, c0:c0 + n], start=True, stop=True), reads=[tabrep, oh], writes=[ps])
                    op("act", lambda a: a.activation(out=fr.t[:, c0:c0 + n], in_=ps.t[:, 0:n], func=AF.Copy), reads=[ps], writes=[fr])
                    c0 += n
                S.dma("sp", FW_d.ap()[h], fr.t[:, 0:LW], reads=[fr], writes=[Fb])
                S.dma("sp", FS_d.ap()[h], fr.t[:, LW:LW + LS], reads=[fr], writes=[Fb])
                S.dma("sp", FC_d.ap()[h], fr.t[:, LW + LS:KF], reads=[fr], writes=[Fb])

            stop_here("s2")

            def toep_load(dst_ap, Fd, L, pstride, off, g, reads, writes):
                hs = 128 * L
                src_ = bass.AP(tensor=Fd, offset=4 * g * hs + off, ap=[[pstride, 128], [hs, 4], [1, 128]])
                S.dma("pool", dst_ap.rearrange("p (b t) -> p b t", b=4), src_, reads=reads, writes=writes)

            for g in range(2):
                for di in range(5):
                    toep_load(WB.t[:, g, di, :], FW_d, LW, LW - 1, di * 128 + 128, g, [Fb], [WB])
                for di in range(3):
                    toep_load(SB.t[:, g, di, :], FS_d, LS, LS - 1, di * 128 + 128, g, [Fb], [SB])
            stop_here("s3")
            cbs = [sbt(st, "cbs%d" % i, [128, 512], BF16) for i in range(2)]
            CBb = Buf("CB")
            for g in range(2):
                for qt in range(NT):
                    cb_ = cbs[(g * NT + qt) % 2]
                    toep_load(cb_.t[:], FC_d, LC, LC - 16, 128 * qt - 16 + 2048, g, [Fb], [cb_])
                    S.dma("sp", CB_d.ap()[g * NT + qt], cb_.t[:], reads=[cb_], writes=[CBb])
            stop_here("s4")
            w1s = sbt(st, "w1s", [64, 32, 256], BF16)
            posb = sbt(st, "posb", [64, 32], BF16)
            for ti, (w1d, pd) in enumerate(((ckw1_d, posk_d), (cvw1_d, posv_d))):
                S.dma("pool", w1s.t[:], w1d.ap().rearrange("(l d) f -> d l f", d=64), writes=[w1s])
                S.dma("pool", posb.t[:], pd.ap()[:, :], writes=[posb])
                for fc in range(2):
                    ps = PS_C.next()
                    for l in range(32):
                        op("pe", lambda t: t.matmul(ps.t[:, 0:1], lhsT=w1s.t[:, l, fc * 128:(fc + 1) * 128], rhs=posb.t[:, l:l + 1], start=(l == 0), stop=(l == 31)), reads=[w1s, posb], writes=[ps])
                    op("act", lambda a: a.activation(out=cbk.t[:, ti * 2 + fc:ti * 2 + fc + 1], in_=ps.t[:, 0:1], func=AF.Copy), reads=[ps], writes=[cbk])
            S.barrier("setup")

        stop_here("s5")
        if "setup" in dbg:
            d1 = ddbg("WB", [128, 2 * 5 * 512]); d2 = ddbg("cbk", [128, 4]); d3 = ddbg("vec", [128, NV])
            with ExitStack() as st:
                tmp = sbt(st, "dbgtmp", [128, 2 * 5 * 512], F32)
                op("dve", lambda v: v.tensor_copy(out=tmp.t[:], in_=WB.t[:].rearrange("p a b c -> p (a b c)")), reads=[WB], writes=[tmp])
                S.dma("sp", d1.ap()[:, :], tmp.t[:], reads=[tmp])
                S.dma("sp", d2.ap()[:, :], cbk.t[:], reads=[cbk])
                S.dma("sp", d3.ap()[:, :], vec.t[:], reads=[vec])
                S.barrier()

        def rstd_inplace(tb, ap, n, extra_reads=()):
            op("act", lambda a: a.activation(out=ap, in_=ap, func=AF.Ln, scale=1.0 / n, bias=EPS[0:ap.shape[0], :]), reads=[tb, vec] + list(extra_reads), writes=[tb])
            op("act", lambda a: a.activation(out=ap, in_=ap, func=AF.Exp, scale=-0.5), reads=[tb], writes=[tb])

        def norm_transpose(src_tb, src_ap, dstT, col0, gcol, scr, ssb, ssi):
            junk, xs16 = scr
            op("act", lambda a: a.activation(out=junk.t[:], in_=src_ap, func=AF.Square, accum_out=ssb.t[:, ssi:ssi + 1]), reads=[src_tb], writes=[junk, ssb])
            rstd_inplace(ssb, ssb.t[:, ssi:ssi + 1], D)
            op("act", lambda a: a.activation(out=xs16.t[:], in_=src_ap, func=AF.Copy, scale=ssb.t[:, ssi:ssi + 1]), reads=[src_tb, ssb], writes=[xs16])
            ps = PS_C.next()
            psv = ps.t[:].bitcast(BF16)
            for kc in range(8):
                op("pe", lambda t: t.transpose(out=psv[:, kc * 128:(kc + 1) * 128], in_=xs16.t[:, kc * 128:(kc + 1) * 128], identity=ident_b.t[:]), reads=[xs16, ident_b], writes=[ps])
            op("dve", lambda v: v.tensor_tensor(out=dstT.t[:, :, col0:col0 + 128], in0=psv.rearrange("p (k t) -> p k t", k=8),
                                                in1=V_(gcol, gcol + 8).unsqueeze(2).to_broadcast([128, 8, 128]), op=ALU.mult), reads=[ps, vec], writes=[dstT])

        moe_bufs = dict(XMb=XMb, SLb=SLb, OUTb=OUTb)
        H1b = Buf("H1")

        for b in range(NB):
            with ExitStack() as eb:
                xT = sbt(eb, "xT", [128, 8, T], BF16)
                mixT = sbt(eb, "mixT", [128, 8, T], BF16)
                with ExitStack() as ea:
                    xs = [sbt(ea, "xs%d" % i, [128, D], F32) for i in range(2)]
                    junk = sbt(ea, "junk", [128, D], BF16)
                    xs16 = sbt(ea, "xs16", [128, D], BF16)
                    ssx = sbt(ea, "ssx", [128, NT], F32)
                    op("dve", lambda v: v.memset(ssx.t[:], 0.0), writes=[ssx])
                    for i in range(NT):
                        xt_ = xs[i % 2]
                        S.dma("sp", xt_.t[:], x_d.ap()[b * T + i * 128: b * T + (i + 1) * 128, :], writes=[xt_])
                        norm_transpose(xt_, xt_.t[:], xT, i * 128, 0, (junk, xs16), ssx, i)
                    if "xT" in dbg and b == 0:
                        S.dma("sp", ddbg("xT", [128, 8 * T], BF16).ap()[:, :], xT.t[:].rearrange("p k t -> p (k t)"), reads=[xT])
                    stop_here("n%d" % len(S.__dict__.setdefault("_nw", [])))
                    S._nw.append(1)
                    wst = [sbt(ea, "wst%d" % i, [128, 8, 512], BF16) for i in range(2)]
                    wring = Ring(wst)
                    winv = win_d.ap().rearrange("(k p) n -> p k n", p=128)

                    def load_w(c0, n, dup64=False):
                        w = wring.next()
                        if dup64:
                            S.dma("pool", w.t[:, :, 0:64], winv[:, :, c0:c0 + 64], writes=[w])
                            S.dma("pool", w.t[:, :, 64:128], winv[:, :, c0:c0 + 64], writes=[w])
                        else:
                            S.dma("pool", w.t[:, :, 0:n], winv[:, :, c0:c0 + n], writes=[w])
                        return w

                    def proj_fm(w, wc0, tb, nco=128):
                        ps = PS_A.next()
                        for kc in range(8):
                            op("pe", lambda t: t.matmul(ps.t[0:nco, :], lhsT=w.t[:, kc, wc0:wc0 + nco], rhs=xT.t[:, kc, tb * 512:(tb + 1) * 512], start=(kc == 0), stop=(kc == 7)), reads=[w, xT], writes=[ps])
                        return ps
                    with ExitStack() as er:
                        B0 = sbt(er, "B0", [128, T + 4], F32)
                        B1 = sbt(er, "B1", [128, T], F32)
                        B2 = sbt(er, "B2", [128, T], BF16)
                        B3 = sbt(er, "B3", [128, T], F32)
                        B4 = sbt(er, "B4", [128, T], F32)
                        B5 = sbt(er, "B5", [128, T], F32)
                        ssa = sbt(er, "ssa", [128, T], F32)
                        TBK = lambda tb: slice(tb * 512, (tb + 1) * 512)
                        for c in range(4):
                            w = load_w(c * 128, 128)
                            op("pool", lambda p: p.memset(B0.t[:, 0:4], 0.0), writes=[B0])
                            for tb in range(4):
                                ps = proj_fm(w, 0, tb)
                                op("act", lambda a: a.activation(out=B0.t[:, 4 + tb * 512: 4 + (tb + 1) * 512], in_=ps.t[:], func=AF.Copy), reads=[ps], writes=[B0])
                            cw = lambda j: V_(32 + c * 4 + j)
                            op("dve", lambda v: v.tensor_scalar(out=B1.t[:], in0=B0.t[:, 4:4 + T], scalar1=cw(3), scalar2=V_(48 + c), op0=ALU.mult, op1=ALU.add), reads=[B0, vec], writes=[B1])
                            for j in (2, 1, 0):
                                op("dve", lambda v: v.scalar_tensor_tensor(out=B1.t[:], in0=B0.t[:, 1 + j:1 + j + T], scalar=cw(j), in1=B1.t[:], op0=ALU.mult, op1=ALU.add), reads=[B0, B1, vec], writes=[B1])
                            op("act", lambda a: a.activation(out=B2.t[:], in_=B1.t[:], func=AF.Copy), reads=[B1], writes=[B2])
                            for tb in range(4):
                                ps = PS_A.next()
                                op("pe", lambda t: t.matmul(ps.t[:], lhsT=wbdr.t[:, c, :], rhs=B2.t[:, TBK(tb)], start=True, stop=True), reads=[wbdr, B2], writes=[ps])
                                op("act", lambda a: a.activation(out=B3.t[:, TBK(tb)], in_=ps.t[:], func=AF.Sigmoid, bias=V_(52 + c)), reads=[ps, vec], writes=[B3])
                                ps = PS_A.next()
                                op("pe", lambda t: t.matmul(ps.t[:], lhsT=wbdi.t[:, c, :], rhs=B2.t[:, TBK(tb)], start=True, stop=True), reads=[wbdi, B2], writes=[ps])
                                op("act", lambda a: a.activation(out=B4.t[:, TBK(tb)], in_=ps.t[:], func=AF.Sigmoid, bias=V_(56 + c)), reads=[ps, vec], writes=[B4])
                            op("act", lambda a: a.activation(out=B5.t[:], in_=B3.t[:], func=AF.Exp, scale=V_(72 + c)), reads=[B3, vec], writes=[B5])
                            op("act", lambda a: a.activation(out=B3.t[:], in_=B3.t[:], func=AF.Exp, scale=V_(76 + c)), reads=[B3, vec], writes=[B3])
                            op("act", lambda a: a.activation(out=B3.t[:], in_=B3.t[:], func=AF.Sqrt, scale=-1.0, bias=1.0), reads=[B3], writes=[B3])
                            op("dve", lambda v: v.tensor_tensor(out=B3.t[:], in0=B3.t[:], in1=B4.t[:], op=ALU.mult), reads=[B3, B4], writes=[B3])
                            op("dve", lambda v: v.tensor_tensor(out=B3.t[:], in0=B3.t[:], in1=B1.t[:], op=ALU.mult), reads=[B3, B1], writes=[B3])
                            op("dve", lambda v: v.tensor_tensor_scan(out=B4.t[:], data0=B5.t[:], data1=B3.t[:], initial=0.0, op0=ALU.mult, op1=ALU.add), reads=[B5, B3], writes=[B4])
                            w = load_w(512 + c * 128, 128)
                            for tb in range(4):
                                ps = proj_fm(w, 0, tb)
                                op("act", lambda a: a.activation(out=B0.t[:, TBK(tb)], in_=ps.t[:], func=AF.Gelu), reads=[ps], writes=[B0])
                            op("dve", lambda v: v.tensor_tensor(out=B1.t[:], in0=B0.t[:, 0:T], in1=B4.t[:], op=ALU.mult), reads=[B0, B4], writes=[B1])
                            op("act", lambda a: a.activation(out=mixT.t[:, c, :], in_=B1.t[:], func=AF.Copy), reads=[B1], writes=[mixT])
                            op("act", lambda a: a.activation(out=B2.t[:], in_=B1.t[:], func=AF.Square), reads=[B1], writes=[B2])
                            for tb in range(4):
                                ps = PS_B.next()
                                op("pe", lambda t: t.matmul(ps.t[:], lhsT=ones_b.t[:], rhs=B2.t[:, TBK(tb)], start=True, stop=True), reads=[ones_b, B2], writes=[ps])
                                if c == 0:
                                    op("dve", lambda v: v.tensor_copy(out=ssa.t[:, TBK(tb)], in_=ps.t[:]), reads=[ps], writes=[ssa])
                                else:
                                    op("dve", lambda v: v.tensor_tensor(out=ssa.t[:, TBK(tb)], in0=ssa.t[:, TBK(tb)], in1=ps.t[:], op=ALU.add), reads=[ps, ssa], writes=[ssa])
                        rstd_inplace(ssa, ssa.t[:], 512)
                        for c in range(4):
                            op("dve", lambda v: v.scalar_tensor_tensor(out=mixT.t[:, c, :], in0=mixT.t[:, c, :], scalar=V_(24 + c), in1=ssa.t[:], op0=ALU.mult, op1=ALU.mult), reads=[mixT, ssa, vec], writes=[mixT])
                    S.barrier("A1")
                if "rg1" in dbg and b == 0:
                    S.dma("sp", ddbg("mixT", [128, 8 * T], BF16).ap()[:, :], mixT.t[:].rearrange("p a b -> p (a b)"), reads=[mixT])
                    S.barrier()
                eb1 = ExitStack()
                qT = sbt(eb1, "qT", [128, 8, T], BF16)
                ksT = sbt(eb1, "ksT", [128, 2, T], BF16)
                kwT = sbt(eb1, "kwT", [128, 2, T], BF16)
                Vs = sbt(eb1, "Vs", [128, NT, 2, 65], BF16)
                Vw = sbt(eb1, "Vw", [128, NT, 2, 65], BF16)
                gates = sbt(eb1, "gates", [128, NT, 24], F32)
                kcmpT = sbt(eb1, "kcmpT", [128, 2, 128], BF16)
                vcmp = sbt(eb1, "vcmp", [128, 2, 97], BF16)
                op("pool", lambda p: p.memset(Vs.t[:, :, :, 64:65], 1.0), writes=[Vs])
                op("pool", lambda p: p.memset(Vw.t[:, :, :, 64:65], 1.0), writes=[Vw])
                op("pool", lambda p: p.memset(kcmpT.t[:], 0.0), writes=[kcmpT])
                qNm = Buf("qNm")
                op("pool", lambda p: p.memset(qT.t[64:128, :, :], 0.0), writes=[qT])
                op("pool", lambda p: p.memset(kwT.t[64:128, :, :], 0.0), writes=[kwT])
                op("pool", lambda p: p.memset(ksT.t[96:128, :, :], 0.0), writes=[ksT])
                for g in range(2):
                    S.dma("pool", ksT.t[64:96, g, :], cd["c_E"].ap()[:, :], writes=[ksT])
                op("pool", lambda p: p.memset(vcmp.t[:], 0.0), writes=[vcmp])
                op("pool", lambda p: p.memset(vcmp.t[:, :, 64:65], 1.0), writes=[vcmp])
                for g in range(2):
                    op("pool", lambda p: p.tensor_copy(out=vcmp.t[:, g, 65:97], in_=ov_b.t[:]), reads=[ov_b], writes=[vcmp])
                with ExitStack() as ea:
                    stop_here("n%d" % len(S.__dict__.setdefault("_nw", [])))
                    S._nw.append(1)
                    wst = [sbt(ea, "wst%d" % i, [128, 8, 512], BF16) for i in range(2)]
                    wring = Ring(wst)
                    winv = win_d.ap().rearrange("(k p) n -> p k n", p=128)

                    def load_w(c0, n, dup64=False):
                        w = wring.next()
                        if dup64:
                            S.dma("pool", w.t[:, :, 0:64], winv[:, :, c0:c0 + 64], writes=[w])
                            S.dma("pool", w.t[:, :, 64:128], winv[:, :, c0:c0 + 64], writes=[w])
                        else:
                            S.dma("pool", w.t[:, :, 0:n], winv[:, :, c0:c0 + n], writes=[w])
                        return w

                    def proj_fm(w, wc0, tb, nco=128):
                        ps = PS_A.next()
                        for kc in range(8):
                            op("pe", lambda t: t.matmul(ps.t[0:nco, :], lhsT=w.t[:, kc, wc0:wc0 + nco], rhs=xT.t[:, kc, tb * 512:(tb + 1) * 512], start=(kc == 0), stop=(kc == 7)), reads=[w, xT], writes=[ps])
                        return ps
                    en2 = ExitStack()
                    q32 = [sbt(en2, "q32_%d" % i, [128, 512], F32) for i in range(2)]
                    qsq = [sbt(en2, "qsq_%d" % i, [128, 512], BF16) for i in range(2)]
                    rsn = [sbt(en2, "rsn_%d" % i, [128, 512], F32) for i in range(2)]
                    nrm_i = [0]

                    hn_pend = []

                    def hn_flush():
                        while hn_pend:
                            a32, asq, ars, dst_ap, dst_tb, gcol = hn_pend.pop(0)
                            p2 = PS_B.next()
                            op("pe", lambda t: t.matmul(p2.t[0:64, :], lhsT=ones_b.t[0:64, 0:64], rhs=asq.t[0:64, :], start=True, stop=True), reads=[ones_b, asq], writes=[p2])
                            op("act", lambda a: a.activation(out=ars.t[0:64, :], in_=p2.t[0:64, :], func=AF.Ln, scale=1.0 / 64, bias=EPS[0:64, :]), reads=[p2, vec], writes=[ars])
                            op("act", lambda a: a.activation(out=ars.t[0:64, :], in_=ars.t[0:64, :], func=AF.Exp, scale=-0.5), reads=[ars], writes=[ars])
                            op("dve", lambda v: v.scalar_tensor_tensor(out=dst_ap, in0=a32.t[0:64, :], scalar=vec.t[0:64, gcol:gcol + 1], in1=ars.t[0:64, :], op0=ALU.mult, op1=ALU.mult), reads=[a32, ars, vec], writes=[dst_tb])

                    def headnorm_store(ps, dst_ap, dst_tb, gcol):
                        hn_flush()
                        k = nrm_i[0] % 2; nrm_i[0] += 1
                        a32, asq, ars = q32[k], qsq[k], rsn[k]
                        op("act", lambda a: a.activation(out=asq.t[0:64, :], in_=ps.t[0:64, :], func=AF.Square), reads=[ps], writes=[asq])
                        op("dve", lambda v: v.tensor_copy(out=a32.t[0:64, :], in_=ps.t[0:64, :]), reads=[ps], writes=[a32])
                        hn_pend.append((a32, asq, ars, dst_ap, dst_tb, gcol))

                    w = load_w(1024, 512)
                    for h in range(8):
                        for tb in range(4):
                            ps = proj_fm(w, h * 64, tb, 64)
                            headnorm_store(ps, qT.t[0:64, h, tb * 512:(tb + 1) * 512], qT, 80)
                    stop_here("a2q")
                    for (c0, dstk, gcol) in ((1792, ksT, 65), (2048, kwT, 66)):
                        w = load_w(c0, 128)
                        for g in range(2):
                            for tb in range(4):
                                ps = proj_fm(w, g * 64, tb, 64)
                                headnorm_store(ps, dstk.t[0:64, g, tb * 512:(tb + 1) * 512], dstk, gcol)
                    hn_flush()
                    S.barrier()
                    en2.close()
                    stop_here("a2k")
                    kcT = sbt(ea, "kcT", [128, T], BF16)
                    vcT = sbt(ea, "vcT", [128, T], BF16)
                    w = load_w(1536, 256)
                    for ci, dst in enumerate((kcT, vcT)):
                        for tb in range(4):
                            ps = proj_fm(w, ci * 128, tb)
                            op("act", lambda a: a.activation(out=dst.t[:, tb * 512:(tb + 1) * 512], in_=ps.t[:], func=AF.Copy), reads=[ps], writes=[dst])
                    stop_here("a2c")
                    w = load_w(1920, 408)
                    for i in range(NT):
                        ps = PS_A.next()
                        for kc in range(8):
                            op("pe", lambda t: t.matmul(ps.t[:, 0:408], lhsT=xT.t[:, kc, i * 128:(i + 1) * 128], rhs=w.t[:, kc, 0:408], start=(kc == 0), stop=(kc == 7)), reads=[w, xT], writes=[ps])
                        op("dve", lambda v: v.tensor_copy(out=Vs.t[:, i, :, 0:64], in_=ps.t[:, 0:128].rearrange("p (g d) -> p g d", g=2)), reads=[ps], writes=[Vs])
                        op("dve", lambda v: v.tensor_copy(out=Vw.t[:, i, :, 0:64], in_=ps.t[:, 256:384].rearrange("p (g d) -> p g d", g=2)), reads=[ps], writes=[Vw])
                        op("act", lambda a: a.activation(out=gates.t[:, i, :], in_=ps.t[:, 384:408], func=AF.Sigmoid), reads=[ps], writes=[gates])
                    stop_here("a2t")
                    with ExitStack() as ec:
                        w1 = sbt(ec, "w1", [128, 32, 256], BF16)
                        w2 = sbt(ec, "w2", [128, 2, 128], BF16)
                        hT = sbt(ec, "hT", [128, 2, 128], BF16)
                        k32 = sbt(ec, "k32", [128, 128], F32)
                        ksq = sbt(ec, "ksq", [128, 128], BF16)
                        krs = sbt(ec, "krs", [128, 128], F32)
                        for ti, (w1d, w2d, srcT) in enumerate(((ckw1_d, ckw2_d, kcT), (cvw1_d, cvw2_d, vcT))):
                            w1v = w1d.ap().rearrange("(l d) f -> d l f", d=64)
                            S.dma("pool", w1.t[0:64], w1v, writes=[w1])
                            S.dma("pool", w1.t[64:128], w1v, writes=[w1])
                            w2v = w2d.ap().rearrange("(c f) d -> f c d", f=128)
                            S.dma("pool", w2.t[:, :, 0:64], w2v, writes=[w2])
                            S.dma("pool", w2.t[:, :, 64:128], w2v, writes=[w2])
                            for g in range(2):
                                pr = slice(g * 64, (g + 1) * 64)
                                ps = PS_A.next()
                                for fc in range(2):
                                    for l in range(32):
                                        op("pe", lambda t: t.matmul(ps.t[:, fc * 128:fc * 128 + 127], lhsT=w1.t[pr, l, fc * 128:(fc + 1) * 128], rhs=srcT.t[pr, l:l + 16 * 126 + 1:16], start=(l == 0), stop=(l == 31)), reads=[w1, srcT], writes=[ps])
                                for fc in range(2):
                                    op("act", lambda a: a.activation(out=hT.t[:, fc, 0:127], in_=ps.t[:, fc * 128:fc * 128 + 127], func=AF.Gelu, bias=cbk.t[:, ti * 2 + fc:ti * 2 + fc + 1]), reads=[ps, cbk], writes=[hT])
                                p2 = PS_B.next()
                                if ti == 0:
                                    for fc in range(2):
                                        op("pe", lambda t: t.matmul(p2.t[:, 0:127], lhsT=w2.t[:, fc, :], rhs=hT.t[:, fc, 0:127], start=(fc == 0), stop=(fc == 1)), reads=[w2, hT], writes=[p2])
                                    op("act", lambda a: a.activation(out=ksq.t[:, 0:127], in_=p2.t[:, 0:127], func=AF.Square), reads=[p2], writes=[ksq])
                                    op("dve", lambda v: v.tensor_copy(out=k32.t[:, 0:127], in_=p2.t[:, 0:127]), reads=[p2], writes=[k32])
                                    p3 = PS_B.next()
                                    op("pe", lambda t: t.matmul(p3.t[:, 0:127], lhsT=onesbd_b.t[:], rhs=ksq.t[:, 0:127], start=True, stop=True), reads=[onesbd_b, ksq], writes=[p3])
                                    op("act", lambda a: a.activation(out=krs.t[:, 0:127], in_=p3.t[:, 0:127], func=AF.Ln, scale=1.0 / 64, bias=EPS), reads=[p3, vec], writes=[krs])
                                    op("act", lambda a: a.activation(out=krs.t[:, 0:127], in_=krs.t[:, 0:127], func=AF.Exp, scale=-0.5), reads=[krs], writes=[krs])
                                    op("dve", lambda v: v.scalar_tensor_tensor(out=kcmpT.t[0:64, g, 0:127], in0=k32.t[0:64, 0:127], scalar=vec.t[0:64, 67:68], in1=krs.t[0:64, 0:127], op0=ALU.mult, op1=ALU.mult), reads=[k32, krs, vec], writes=[kcmpT])
                                else:
                                    for fc in range(2):
                                        op("pe", lambda t: t.matmul(p2.t[0:127, 0:64], lhsT=hT.t[:, fc, 0:127], rhs=w2.t[:, fc, 0:64], start=(fc == 0), stop=(fc == 1)), reads=[w2, hT], writes=[p2])
                                    op("act", lambda a: a.activation(out=vcmp.t[0:127, g, 0:64], in_=p2.t[0:127, 0:64], func=AF.Copy), reads=[p2], writes=[vcmp])
                    if "cmp" in dbg and b == 0:
                        S.dma("sp", ddbg("kcmpT", [64, 256], BF16).ap()[:, :], kcmpT.t[:].rearrange("p a b -> p (a b)"), reads=[kcmpT])
                        S.dma("sp", ddbg("vcmp", [128, 2 * 97], BF16).ap()[:, :], vcmp.t[:].rearrange("p a b -> p (a b)"), reads=[vcmp])
                        S.dma("sp", ddbg("qT", [64, 8 * T], BF16).ap()[:, :], qT.t[:].rearrange("p a b -> p (a b)"), reads=[qT])
                        S.dma("sp", ddbg("kwT", [64, 2 * T], BF16).ap()[:, :], kwT.t[:].rearrange("p a b -> p (a b)"), reads=[kwT])
                        S.dma("sp", ddbg("Vw", [128, NT * 2 * 65], BF16).ap()[:, :], Vw.t[:].rearrange("p a b c -> p (a b c)"), reads=[Vw])
                        S.dma("sp", ddbg("gates", [128, NT * 24], F32).ap()[:, :], gates.t[:].rearrange("p a b -> p (a b)"), reads=[gates])

                    S.barrier("A2")
                if "rg" in dbg and b == 0:
                    S.dma("sp", ddbg("mixT", [128, 8 * T], BF16).ap()[:, :], mixT.t[:].rearrange("p a b -> p (a b)"), reads=[mixT])
                if stop_after == "A":
                    S.barrier()
                    eb1.close()
                    continue

                with ExitStack() as eat:
                    ynsa = TB(xT.t[:].rearrange("p k t -> p (k t)").bitcast(F32).rearrange("p (a c) -> p a c", a=NT), "ynsa")
                    ynsa.b = xT.b
                    nmp = [sbt(eat, "nmp%d" % i, [128, 128], BF16) for i in range(2)]
                    for n_ in nmp:
                        op("pool", lambda p: p.memset(n_.t[:], 0.0), writes=[n_])
                    cbt = [sbt(eat, "cbt%d" % i, [128, 512], BF16) for i in range(2)]
                    pTs = Ring([sbt(eat, "pT%d" % i, [128, 512], BF16) for i in range(4)])
                    sm = Ring([sbt(eat, "sm%d" % i, [128, 96], F32) for i in range(4)])
                    otmp = Ring([sbt(eat, "otmp%d" % i, [128, 4, 64], F32) for i in range(3)])
                    itmp = [sbt(eat, "itmp%d" % i, [128, 4, 32], F32) for i in range(2)]
                    imp = [sbt(eat, "imp%d" % i, [128, 32], F32) for i in range(2)]
                    nm = sbt(eat, "nm", [128, 32], F32)

                    def qk_scores(ps, kT, g, kt, qt, first):
                        ks_ = slice(kt * 128, (kt + 1) * 128); qs_ = slice(qt * 128, (qt + 1) * 128)
                        op("pe", lambda t: t.matmul(ps.t[:].rearrange("p (a t) -> p a t", a=4), lhsT=kT[:, g, ks_] if kT is not None else kcmpT.t[:, g, :],
                                                    rhs=qT.t[:, 4 * g:4 * g + 4, qs_], start=False, stop=True), reads=[qT] + first, writes=[ps])

                    def finish_branch(po, ncol, g, qt, br, first):
                        pov = po.t[:, 0:4 * ncol].rearrange("p (s c) -> p s c", s=4)
                        s_ = sm.next()
                        rden = s_.t[:, 0:4]; wv = s_.t[:, 4:8]
                        op("dve", lambda v: v.tensor_scalar(out=rden, in0=pov[:, :, 64], scalar1=1e-30, scalar2=None, op0=ALU.add), reads=[po], writes=[s_])
                        op("dve", lambda v: v.reciprocal(out=rden, in_=rden), reads=[s_], writes=[s_])
                        gv = gates.t[:, qt, 12 * g:12 * g + 12].rearrange("p (s b) -> p s b", s=4)[:, :, br]
                        op("dve", lambda v: v.tensor_tensor(out=wv, in0=rden, in1=gv, op=ALU.mult), reads=[s_, gates], writes=[s_])
                        yv = ynsa.t[:, qt, g * 256:(g + 1) * 256].rearrange("p (s d) -> p s d", s=4)
                        if first:
                            op("dve", lambda v: v.tensor_tensor(out=yv, in0=pov[:, :, 0:64], in1=wv.unsqueeze(2).to_broadcast([128, 4, 64]), op=ALU.mult), reads=[po, s_], writes=[ynsa])
                        else:
                            o_ = otmp.next()
                            op("dve", lambda v: v.tensor_tensor(out=o_.t[:], in0=pov[:, :, 0:64], in1=wv.unsqueeze(2).to_broadcast([128, 4, 64]), op=ALU.mult), reads=[po, s_], writes=[o_])
                            op("pool", lambda p: p.tensor_tensor(out=yv, in0=yv, in1=o_.t[:], op=ALU.add), reads=[o_, ynsa], writes=[ynsa])
                        return s_

                    cmp_units = [(g, qt) for g in range(2) for qt in range(NT)]

                    def cmp_scores(u):
                        g, qt = cmp_units[u]
                        cb_ = cbt[u % 2]
                        S.dma("sp", cb_.t[:], CB_d.ap()[g * NT + qt], reads=[CBb], writes=[cb_])
                        ps = PS_A.next()
                        op("pe", lambda t: t.matmul(ps.t[:], lhsT=ident_b.t[:], rhs=cb_.t[:], start=True, stop=False), reads=[ident_b, cb_], writes=[ps])
                        qk_scores(ps, None, g, 0, qt, [kcmpT])
                        pT = pTs.next()
                        op("act", lambda a: a.activation(out=pT.t[:], in_=ps.t[:], func=AF.Exp), reads=[ps], writes=[pT])
                        return pT

                    def cmp_pv(u, pT):
                        g, qt = cmp_units[u]
                        po = PS_B.next()
                        for sl in range(4):
                            op("pe", lambda t: t.matmul(po.t[:, sl * 97:(sl + 1) * 97], lhsT=pT.t[:, sl * 128:(sl + 1) * 128], rhs=vcmp.t[:, g, :], start=(sl == 0), stop=True, skip_group_check=True), reads=[pT, vcmp], writes=[po])
                        s_ = finish_branch(po, 97, g, qt, 0, True)
                        it_ = itmp[u % 2]; im_ = imp[u % 2]
                        pov = po.t[:, 0:4 * 97].rearrange("p (s c) -> p s c", s=4)
                        op("dve", lambda v: v.tensor_tensor(out=it_.t[:], in0=pov[:, :, 65:97], in1=s_.t[:, 0:4].unsqueeze(2).to_broadcast([128, 4, 32]), op=ALU.mult), reads=[po, s_], writes=[it_])
                        op("dve", lambda v: v.tensor_reduce(out=im_.t[:], in_=it_.t[:].rearrange("p s j -> p j s"), axis=AX.X, op=ALU.add), reads=[it_], writes=[im_])
                        op("dve", lambda v: v.tensor_tensor(out=im_.t[:], in0=im_.t[:], in1=keep_t.t[:, qt, :], op=ALU.mult), reads=[im_, keep_t], writes=[im_])
                        op("dve", lambda v: v.tensor_tensor(out=im_.t[:], in0=im_.t[:], in1=addm_t.t[:, qt, :], op=ALU.add), reads=[im_, addm_t], writes=[im_])
                        op("dve", lambda v: v.max(out=s_.t[:, 8:16], in_=im_.t[:]), reads=[im_], writes=[s_])
                        nm_ = nmp[u % 2]
                        op("dve", lambda v: v.tensor_scalar(out=nm_.t[:, 64:96], in0=im_.t[:], scalar1=s_.t[:, 15:16], scalar2=NEG, op0=ALU.is_lt, op1=ALU.mult), reads=[im_, s_], writes=[nm_])
                        return nm_

                    def cmp_tail(u, nm_):
                        g, qt = cmp_units[u]
                        pt_ = PS_C.next()
                        op("pe", lambda t: t.matmul(pt_.t[:, 0:128], lhsT=nm_.t[:], rhs=ident_b.t[:], start=True, stop=True), reads=[nm_, ident_b], writes=[pt_])
                        op("act", lambda a: a.activation(out=qT.t[64:96, 4 * g:4 * g + 4, qt * 128:(qt + 1) * 128], in_=pt_.t[64:96, 0:128].unsqueeze(1).to_broadcast([32, 4, 128]), func=AF.Copy), reads=[pt_], writes=[qNm])

                    nU = len(cmp_units)
                    pTq = {0: cmp_scores(0)}
                    nmq = {}
                    for u in range(nU):
                        if u + 1 < nU:
                            pTq[u + 1] = cmp_scores(u + 1)
                        nmq[u] = cmp_pv(u, pTq.pop(u))
                        if u >= 1:
                            cmp_tail(u - 1, nmq.pop(u - 1))
                    cmp_tail(nU - 1, nmq.pop(nU - 1))
                    if "nm" in dbg and b == 0:
                        S.dma("sp", ddbg("ynsa_c", [128, NT * 512], F32).ap()[:, :], ynsa.t[:].rearrange("p a b -> p (a b)"), reads=[ynsa])

                    MARKS.append(("Bcmp", dict(S.cnt)))
                    n_cv = (3 * NEXP + NB - 1) // NB
                    for ci in range(b * n_cv, min(3 * NEXP, (b + 1) * n_cv)):
                        wi, e_ = ci % 3, ci // 3
                        src_ = (ew1_d, ew3_d, ew2_d)[wi].ap()[e_].rearrange("(p k) n -> p (k n)", p=128)
                        S.dma("pool", EWB_d[wi].ap()[e_ * 128:(e_ + 1) * 128, :], src_, writes=[Buf()])
                    jobs = []
                    for qt in range(NT):
                        for g in range(2):
                            for kt in range(qt + 1):
                                jobs.append(("sel", g, qt, kt, kt == 0, kt == qt))
                            k0 = max(0, qt - 4)
                            for kt in range(k0, qt + 1):
                                jobs.append(("win", g, qt, kt, kt == k0, kt == qt))
                    pos_ = {}

                    def emit_scores(job):
                        kind, g, qt, kt, first, last = job
                        ps = PS_A.next()
                        if kind == "sel":
                            di = min(qt - kt, 2)
                            op("pe", lambda t: t.matmul(ps.t[:], lhsT=ident_b.t[:], rhs=SB.t[:, g, di, :], start=True, stop=False), reads=[ident_b, SB], writes=[ps])
                            qk_scores(ps, ksT.t, g, kt, qt, [ksT, qNm])
                        else:
                            di = qt - kt
                            op("pe", lambda t: t.matmul(ps.t[:], lhsT=ident_b.t[:], rhs=WB.t[:, g, di, :], start=True, stop=False), reads=[ident_b, WB], writes=[ps])
                            qk_scores(ps, kwT.t, g, kt, qt, [kwT])
                        return ps

                    def emit_rest(job, ps):
                        kind, g, qt, kt, first, last = job
                        pT = pTs.next()
                        op("act", lambda a: a.activation(out=pT.t[:], in_=ps.t[:], func=AF.Exp), reads=[ps], writes=[pT])
                        if first:
                            pos_[(kind, g, qt)] = PS_B.next()
                        po = pos_[(kind, g, qt)]
                        Vt = Vs if kind == "sel" else Vw
                        for sl in range(4):
                            op("pe", lambda t: t.matmul(po.t[:, sl * 65:(sl + 1) * 65], lhsT=pT.t[:, sl * 128:(sl + 1) * 128], rhs=Vt.t[:, kt, g, :], start=(first and sl == 0), stop=last, skip_group_check=True), reads=[pT, Vt], writes=[po])
                        if last:
                            finish_branch(po, 65, g, qt, 1 if kind == "sel" else 2, False)
                            del pos_[(kind, g, qt)]

                    pend = None
                    for job in jobs:
                        ps = emit_scores(job)
                        if pend is not None:
                            emit_rest(*pend)
                        pend = (job, ps)
                    emit_rest(*pend)
                    if "ynsa" in dbg and b == 0:
                        S.dma("sp", ddbg("ynsa", [128, NT * 512], F32).ap()[:, :], ynsa.t[:].rearrange("p a b -> p (a b)"), reads=[ynsa])
                    MARKS.append(("Bsw", dict(S.cnt)))
                    junk2 = sbt(eat, "junk2", [128, 512], BF16)
                    yn16 = sbt(eat, "yn16", [128, 512], BF16)
                    ssy = sbt(eat, "ssy", [128, NT], F32)
                    op("dve", lambda v: v.memset(ssy.t[:], 0.0), writes=[ssy])
                    for qt in range(NT):
                        op("act", lambda a: a.activation(out=junk2.t[:], in_=ynsa.t[:, qt, :], func=AF.Square, accum_out=ssy.t[:, qt:qt + 1]), reads=[ynsa], writes=[junk2, ssy])
                        rstd_inplace(ssy, ssy.t[:, qt:qt + 1], 512)
                        op("act", lambda a: a.activation(out=yn16.t[:], in_=ynsa.t[:, qt, :], func=AF.Copy, scale=ssy.t[:, qt:qt + 1]), reads=[ynsa, ssy], writes=[yn16])
                        ps = PS_C.next()
                        psv = ps.t[:].bitcast(BF16)
                        for cc in range(4):
                            op("pe", lambda t: t.transpose(out=psv[:, cc * 128:(cc + 1) * 128], in_=yn16.t[:, cc * 128:(cc + 1) * 128], identity=ident_b.t[:]), reads=[yn16, ident_b], writes=[ps])
                        op("dve", lambda v: v.tensor_tensor(out=mixT.t[:, 4:8, qt * 128:(qt + 1) * 128], in0=psv[:, 0:512].rearrange("p (k t) -> p k t", k=4),
                                                            in1=V_(28, 32).unsqueeze(2).to_broadcast([128, 4, 128]), op=ALU.mult), reads=[ps, vec], writes=[mixT])
                    S.barrier("B")
                eb1.close()
                if stop_after == "B":
                    S.barrier()
                    continue

                with ExitStack() as ecx:
                    wo = sbt(ecx, "wo", [128, 8, D], BF16)
                    S.dma("pool", wo.t[:], wout_d.ap().rearrange("(k p) n -> p k n", p=128), writes=[wo])
                    xs = [sbt(ecx, "xr%d" % i, [128, D], F32) for i in range(2)]
                    junk = sbt(ecx, "junkc", [128, D], BF16)
                    xs16 = sbt(ecx, "xs16c", [128, D], BF16)
                    ssx = sbt(ecx, "ssxc", [128, NT], F32)
                    op("dve", lambda v: v.memset(ssx.t[:], 0.0), writes=[ssx])
                    for i in range(NT):
                        xt_ = xs[i % 2]
                        rows = slice(b * T + i * 128, b * T + (i + 1) * 128)
                        S.dma("sp", xt_.t[:], x_d.ap()[rows, :], writes=[xt_])
                        for hf in range(2):
                            ps = PS_A.next()
                            for cc in range(8):
                                op("pe", lambda t: t.matmul(ps.t[:], lhsT=mixT.t[:, cc, i * 128:(i + 1) * 128], rhs=wo.t[:, cc, hf * 512:(hf + 1) * 512], start=(cc == 0), stop=(cc == 7)), reads=[mixT, wo], writes=[ps])
                            op("dve", lambda v: v.tensor_tensor(out=xt_.t[:, hf * 512:(hf + 1) * 512], in0=xt_.t[:, hf * 512:(hf + 1) * 512], in1=ps.t[:], op=ALU.add), reads=[ps, xt_], writes=[xt_])
                        S.dma("sp", H1_d.ap()[rows, :], xt_.t[:], reads=[xt_], writes=[Buf()])
                        norm_transpose(xt_, xt_.t[:], xT, i * 128, 8, (junk, xs16), ssx, i)
                    S.barrier("C")
                if "h1" in dbg and b == 0:
                    with ExitStack() as st:
                        tmp = sbt(st, "dbgh1", [128, NT, D], F32)
                        S.dma("sp", tmp.t[:], H1_d.ap()[0:T, :].rearrange("(i p) d -> p i d", p=128), reads=[H1b], writes=[tmp])
                        S.dma("sp", ddbg("h1", [T, D]).ap().rearrange("(i p) d -> p i d", p=128), tmp.t[:], reads=[tmp])
                        S.barrier()
                if stop_after == "C":
                    S.barrier()
                    continue

                with ExitStack() as ed:
                    OT = mixT
                    with ExitStack() as ed1:
                        mT = sbt(ed1, "mT", [128, 8, ML], BF16)
                        kxT = sbt(ed1, "kxT", [128, 8, ML], BF16)
                        Vx = sbt(ed1, "Vx", [128, 2, D], BF16)
                        xs = [sbt(ed1, "xm%d" % i, [128, D], F32) for i in range(2)]
                        junk = sbt(ed1, "junkd", [128, D], BF16)
                        xs16 = sbt(ed1, "xs16d", [128, D], BF16)
                        ssx = sbt(ed1, "ssxd", [128, 2], F32)
                        op("dve", lambda v: v.memset(ssx.t[:], 0.0), writes=[ssx])
                        for i in range(2):
                            S.dma("sp", xs[i].t[:], mem_d.ap()[b * ML + i * 128: b * ML + (i + 1) * 128, :], writes=[xs[i]])
                            norm_transpose(xs[i], xs[i].t[:], mT, i * 128, 16, (junk, xs16), ssx, i)
                        wkv = Ring([sbt(ed1, "wkv%d" % i, [128, 8, 512], BF16) for i in range(2)])
                        wkvv = xwkv_d.ap().rearrange("(k p) n -> p k n", p=128)
                        k32 = [sbt(ed1, "kx32_%d" % i, [128, ML], F32) for i in range(2)]
                        ksq = sbt(ed1, "kxsq", [128, ML], BF16)
                        krs = sbt(ed1, "kxrs", [128, ML], F32)
                        for grp in range(2):
                            w = wkv.next()
                            S.dma("pool", w.t[:], wkvv[:, :, grp * 512:(grp + 1) * 512], writes=[w])
                            for hl in range(2):
                                h = grp * 2 + hl
                                pss = PS_B.next()
                                for dc in range(2):
                                    ps = PS_A.next()
                                    for kc in range(8):
                                        op("pe", lambda t: t.matmul(ps.t[:, 0:ML], lhsT=w.t[:, kc, (hl * 2 + dc) * 128:(hl * 2 + dc + 1) * 128], rhs=mT.t[:, kc, :], start=(kc == 0), stop=(kc == 7)), reads=[w, mT], writes=[ps])
                                    op("act", lambda a: a.activation(out=ksq.t[:], in_=ps.t[:, 0:ML], func=AF.Square), reads=[ps], writes=[ksq])
                                    op("dve", lambda v: v.tensor_copy(out=k32[dc].t[:], in_=ps.t[:, 0:ML]), reads=[ps], writes=[k32[dc]])
                                    op("pe", lambda t: t.matmul(pss.t[:, 0:ML], lhsT=ones_b.t[:], rhs=ksq.t[:], start=(dc == 0), stop=(dc == 1)), reads=[ones_b, ksq], writes=[pss])
                                op("act", lambda a: a.activation(out=krs.t[:], in_=pss.t[:, 0:ML], func=AF.Ln, scale=1.0 / 256, bias=EPS), reads=[pss, vec], writes=[krs])
                                op("act", lambda a: a.activation(out=krs.t[:], in_=krs.t[:], func=AF.Exp, scale=-0.5), reads=[krs], writes=[krs])
                                for dc in range(2):
                                    op("dve", lambda v: v.scalar_tensor_tensor(out=kxT.t[:, h * 2 + dc, :], in0=k32[dc].t[:], scalar=V_(70 + dc), in1=krs.t[:], op0=ALU.mult, op1=ALU.mult), reads=[k32[dc], krs, vec], writes=[kxT])
                        for hf in range(2):
                            w = wkv.next()
                            S.dma("pool", w.t[:], wkvv[:, :, D + hf * 512: D + (hf + 1) * 512], writes=[w])
                            for mt in range(2):
                                ps = PS_A.next()
                                for kc in range(8):
                                    op("pe", lambda t: t.matmul(ps.t[:], lhsT=mT.t[:, kc, mt * 128:(mt + 1) * 128], rhs=w.t[:, kc, :], start=(kc == 0), stop=(kc == 7)), reads=[w, mT], writes=[ps])
                                op("act", lambda a: a.activation(out=Vx.t[:, mt, hf * 512:(hf + 1) * 512], in_=ps.t[:], func=AF.Copy), reads=[ps], writes=[Vx])
                        wqr = Ring([sbt(ed1, "wq%d" % i, [128, 8, 256], BF16) for i in range(2)])
                        wqv = xwq_d.ap().rearrange("(k p) n -> p k n", p=128)
                        NS = 3
                        qx = [sbt(ed1, "qx%d" % i, [128, 2, 512], BF16) for i in range(NS)]
                        q32x = [[sbt(ed1, "q32x%d_%d" % (i, dc), [128, 512], F32) for dc in range(2)] for i in range(NS)]
                        qsqx = [[sbt(ed1, "qsqx%d_%d" % (i, dc), [128, 512], BF16) for dc in range(2)] for i in range(NS)]
                        qrs = [sbt(ed1, "qrs%d" % i, [128, 512], F32) for i in range(NS)]
                        pTx = [[sbt(ed1, "pTx%d_%d" % (i, mt), [128, 512], BF16) for mt in range(2)] for i in range(NS)]
                        rdn = [sbt(ed1, "rdn%d" % i, [128, 512], F32) for i in range(NS)]
                        units = [(h, tb) for h in range(4) for tb in range(4)]
                        wq_of = {}

                        def dA(u):
                            h, tb = units[u]
                            if tb == 0:
                                w = wqr.next()
                                S.dma("pool", w.t[:], wqv[:, :, h * 256:(h + 1) * 256], writes=[w])
                                wq_of[h] = w
                            w = wq_of[h]
                            k_ = u % NS
                            for dc in range(2):
                                ps = PS_A.next()
                                for kc in range(8):
                                    op("pe", lambda t: t.matmul(ps.t[:], lhsT=w.t[:, kc, dc * 128:(dc + 1) * 128], rhs=xT.t[:, kc, tb * 512:(tb + 1) * 512], start=(kc == 0), stop=(kc == 7)), reads=[w, xT], writes=[ps])
                                op("act", lambda a: a.activation(out=qsqx[k_][dc].t[:], in_=ps.t[:], func=AF.Square), reads=[ps], writes=[qsqx[k_][dc]])
                                op("dve", lambda v: v.tensor_copy(out=q32x[k_][dc].t[:], in_=ps.t[:]), reads=[ps], writes=[q32x[k_][dc]])

                        def dB(u):
                            h, tb = units[u]
                            k_ = u % NS
                            pss = PS_B.next()
                            for dc in range(2):
                                op("pe", lambda t: t.matmul(pss.t[:], lhsT=ones_b.t[:], rhs=qsqx[k_][dc].t[:], start=(dc == 0), stop=(dc == 1)), reads=[ones_b, qsqx[k_][dc]], writes=[pss])
                            op("act", lambda a: a.activation(out=qrs[k_].t[:], in_=pss.t[:], func=AF.Ln, scale=1.0 / 256, bias=EPS), reads=[pss, vec], writes=[qrs[k_]])
                            op("act", lambda a: a.activation(out=qrs[k_].t[:], in_=qrs[k_].t[:], func=AF.Exp, scale=-0.5), reads=[qrs[k_]], writes=[qrs[k_]])
                            for dc in range(2):
                                op("dve", lambda v: v.scalar_tensor_tensor(out=qx[k_].t[:, dc, :], in0=q32x[k_][dc].t[:], scalar=V_(81 + dc), in1=qrs[k_].t[:], op0=ALU.mult, op1=ALU.mult), reads=[q32x[k_][dc], qrs[k_], vec], writes=[qx[k_]])

                        def dC(u):
                            h, tb = units[u]
                            k_ = u % NS
                            for mt in range(2):
                                ps = PS_A.next()
                                for dc in range(2):
                                    op("pe", lambda t: t.matmul(ps.t[:], lhsT=kxT.t[:, h * 2 + dc, mt * 128:(mt + 1) * 128], rhs=qx[k_].t[:, dc, :], start=(dc == 0), stop=(dc == 1)), reads=[kxT, qx[k_]], writes=[ps])
                                op("act", lambda a: a.activation(out=pTx[k_][mt].t[:], in_=ps.t[:], func=AF.Exp), reads=[ps], writes=[pTx[k_][mt]])

                        def dD(u):
                            h, tb = units[u]
                            k_ = u % NS
                            pts = pTx[k_]
                            pd = PS_B.next()
                            for mt in range(2):
                                op("pe", lambda t: t.matmul(pd.t[:], lhsT=ones_b.t[:], rhs=pts[mt].t[:], start=(mt == 0), stop=(mt == 1)), reads=[ones_b, pts[mt]], writes=[pd])
                            op("act", lambda a: a.activation(out=rdn[k_].t[:], in_=pd.t[:], func=AF.Ln), reads=[pd], writes=[rdn[k_]])
                            op("act", lambda a: a.activation(out=rdn[k_].t[:], in_=rdn[k_].t[:], func=AF.Exp, scale=-1.0), reads=[rdn[k_]], writes=[rdn[k_]])
                            for dc in range(2):
                                po = PS_B.next()
                                for mt in range(2):
                                    op("pe", lambda t: t.matmul(po.t[:], lhsT=Vx.t[:, mt, h * 256 + dc * 128: h * 256 + (dc + 1) * 128], rhs=pts[mt].t[:], start=(mt == 0), stop=(mt == 1)), reads=[Vx, pts[mt]], writes=[po])
                                op("dve", lambda v: v.tensor_tensor(out=OT.t[:, h * 2 + dc, tb * 512:(tb + 1) * 512], in0=po.t[:], in1=rdn[k_].t[:], op=ALU.mult), reads=[po, rdn[k_]], writes=[OT])

                        nU = len(units)
                        for it in range(nU + 2):
                            if it < nU:
                                dA(it)
                            if 1 <= it <= nU:
                                dB(it - 1)
                            if it >= 2:
                                dD(it - 2)
                            if 1 <= it <= nU:
                                dC(it - 1)
                        S.barrier("D")
                    with ExitStack() as ed2:
                        wo = sbt(ed2, "wxo", [128, 8, D], BF16)
                        S.dma("pool", wo.t[:], xwo_d.ap().rearrange("(k p) n -> p k n", p=128), writes=[wo])
                        hs = [sbt(ed2, "h2_%d" % i, [128, D], F32) for i in range(2)]
                        junk = sbt(ed2, "junke", [128, D], BF16)
                        xm32s = [sbt(ed2, "xm32_%d" % i, [128, D], F32) for i in range(2)]
                        xm16 = [sbt(ed2, "xm16_%d" % i, [128, D], BF16) for i in range(2)]
                        xmTs = [sbt(ed2, "xmT%d" % i, [128, 8, 128], F32) for i in range(2)]
                        ssx = sbt(ed2, "ssxe", [128, NT], F32)
                        lgs = [sbt(ed2, "lg%d" % i, [128, 36], F32) for i in range(2)]
                        rts = [sbt(ed2, "rt%d" % i, [128, 256], F32) for i in range(3)]
                        ohss = [sbt(ed2, "ohs%d" % i, [128, 32], BF16) for i in range(3)]
                        op("dve", lambda v: v.memset(ssx.t[:], 0.0), writes=[ssx])
                        def stage1(i):
                            ht = hs[i % 2]
                            xm32 = xm32s[i % 2]
                            rows = slice(b * T + i * 128, b * T + (i + 1) * 128)
                            S.dma("sp", ht.t[:], H1_d.ap()[rows, :], writes=[ht])
                            for hf in range(2):
                                ps = PS_A.next()
                                for cc in range(8):
                                    op("pe", lambda t: t.matmul(ps.t[:], lhsT=OT.t[:, cc, i * 128:(i + 1) * 128], rhs=wo.t[:, cc, hf * 512:(hf + 1) * 512], start=(cc == 0), stop=(cc == 7)), reads=[OT, wo], writes=[ps])
                                op("dve", lambda v: v.tensor_tensor(out=ht.t[:, hf * 512:(hf + 1) * 512], in0=ht.t[:, hf * 512:(hf + 1) * 512], in1=ps.t[:], op=ALU.add), reads=[ps, ht], writes=[ht])
                            S.dma("sp", out_d.ap()[rows, :], ht.t[:], reads=[ht], writes=[Buf()])
                            op("act", lambda a: a.activation(out=junk.t[:], in_=ht.t[:], func=AF.Square, accum_out=ssx.t[:, i:i + 1]), reads=[ht], writes=[junk, ssx])
                            rstd_inplace(ssx, ssx.t[:, i:i + 1], D)
                            op("dve", lambda v: v.scalar_tensor_tensor(out=xm32.t[:], in0=ht.t[:], scalar=ssx.t[:, i:i + 1], in1=gmoe_t.t[:], op0=ALU.mult, op1=ALU.mult), reads=[ht, ssx, gmoe_t], writes=[xm32])
                            x16 = xm16[i % 2]
                            op("act", lambda a: a.activation(out=x16.t[:], in_=xm32.t[:], func=AF.Copy), reads=[xm32], writes=[x16])
                            S.dma("sp", XM_d.ap()[rows, :], x16.t[:], reads=[x16], writes=[Buf()])

                        def stage2(i):
                            xm32 = xm32s[i % 2]; xmT = xmTs[i % 2]; lg = lgs[i % 2]; ohs = ohss[i % 3]
                            for half in range(2):
                                ps = PS_C.next()
                                for k4 in range(4):
                                    kc = half * 4 + k4
                                    op("pe", lambda t: t.transpose(out=ps.t[:, k4 * 128:(k4 + 1) * 128], in_=xm32.t[:, kc * 128:(kc + 1) * 128], identity=ident_f.t[:]), reads=[xm32, ident_f], writes=[ps])
                                op("act", lambda a: a.activation(out=xmT.t[:, half * 4:(half + 1) * 4, :], in_=ps.t[:].rearrange("p (k t) -> p k t", k=4), func=AF.Copy), reads=[ps], writes=[xmT])
                            ps = PS_C.next()
                            for kc in range(8):
                                op("pe", lambda t: t.matmul(ps.t[:, 0:36], lhsT=xmT.t[:, kc, :], rhs=wr_t.t[:, kc, :], start=(kc == 0), stop=(kc == 7)), reads=[xmT, wr_t], writes=[ps])
                            op("dve", lambda v: v.tensor_tensor(out=lg.t[:], in0=ps.t[:, 0:36], in1=brc_t.t[:], op=ALU.add), reads=[ps, brc_t], writes=[lg])
                            r = rts[i % 3]
                            R_ = lambda a, b_=None: r.t[:, a:(a + 1 if b_ is None else b_)]
                            op("dve", lambda v: v.tensor_reduce(out=R_(0), in_=lg.t[:, 0:4], axis=AX.X, op=ALU.max), reads=[lg], writes=[r])
                            op("dve", lambda v: v.tensor_scalar(out=R_(1), in0=R_(0), scalar1=-1.0, scalar2=None, op0=ALU.mult), reads=[r], writes=[r])
                            op("dve", lambda v: v.tensor_scalar(out=R_(4, 8), in0=lg.t[:, 0:4], scalar1=R_(0), scalar2=None, op0=ALU.is_equal), reads=[lg, r], writes=[r])
                            op("dve", lambda v: v.memset(R_(2), 0.0), reads=[r], writes=[r])
                            op("act", lambda a: a.activation(out=R_(8, 12), in_=lg.t[:, 0:4], func=AF.Exp, bias=R_(1), accum_out=R_(2)), reads=[lg, r], writes=[r])
                            op("dve", lambda v: v.reciprocal(out=R_(3), in_=R_(2)), reads=[r], writes=[r])
                            op("dve", lambda v: v.tensor_tensor(out=R_(16, 48).rearrange("p (g e) -> p g e", g=4), in0=lg.t[:, 4:36].rearrange("p (g e) -> p g e", g=4),
                                                                in1=R_(4, 8).unsqueeze(2).to_broadcast([128, 4, 8]), op=ALU.mult), reads=[lg, r], writes=[r])
                            op("dve", lambda v: v.tensor_reduce(out=R_(48, 56), in_=R_(16, 48).rearrange("p (g e) -> p e g", g=4), axis=AX.X, op=ALU.add), reads=[r], writes=[r])
                            op("dve", lambda v: v.max(out=R_(56, 64), in_=R_(48, 56)), reads=[r], writes=[r])
                            op("dve", lambda v: v.tensor_tensor(out=R_(64), in0=R_(57), in1=R_(56), op=ALU.subtract), reads=[r], writes=[r])
                            op("act", lambda a: a.activation(out=R_(65), in_=R_(64), func=AF.Exp), reads=[r], writes=[r])
                            op("dve", lambda v: v.tensor_scalar(out=R_(66), in0=R_(65), scalar1=1.0, scalar2=None, op0=ALU.add), reads=[r], writes=[r])
                            op("dve", lambda v: v.reciprocal(out=R_(66), in_=R_(66)), reads=[r], writes=[r])
                            op("dve", lambda v: v.tensor_tensor(out=R_(67), in0=R_(65), in1=R_(66), op=ALU.mult), reads=[r], writes=[r])
                            col = (b * NT + i) * 2
                            op("dve", lambda v: v.tensor_scalar(out=REC.t[:, col:col + 2, 1], in0=R_(66, 68), scalar1=R_(3), scalar2=None, op0=ALU.mult), reads=[r], writes=[REC])
                            op("dve", lambda v: v.tensor_copy(out=REC.t[:, col:col + 2, 0], in_=tokid_t.t[:, b * NT + i: b * NT + i + 1].to_broadcast([128, 2])), reads=[tokid_t], writes=[REC])
                            op("dve", lambda v: v.tensor_scalar(out=R_(68, 76), in0=R_(48, 56), scalar1=R_(56), scalar2=None, op0=ALU.is_equal), reads=[r], writes=[r])
                            op("dve", lambda v: v.tensor_scalar(out=R_(76, 84), in0=R_(48, 56), scalar1=R_(57), scalar2=None, op0=ALU.is_equal), reads=[r], writes=[r])
                            for k in range(2):
                                op("dve", lambda v: v.tensor_tensor(out=R_(96 + 32 * k, 128 + 32 * k).rearrange("p (g e) -> p g e", g=4), in0=R_(4, 8).unsqueeze(2).to_broadcast([128, 4, 8]),
                                                                    in1=R_(68 + 8 * k, 76 + 8 * k).unsqueeze(1).to_broadcast([128, 4, 8]), op=ALU.mult), reads=[r], writes=[r])
                            op("dve", lambda v: v.tensor_tensor(out=ohs.t[:], in0=R_(96, 128), in1=R_(128, 160), op=ALU.add), reads=[r], writes=[ohs])

                        def stage3(i):
                            r = rts[i % 3]; ohs = ohss[i % 3]
                            R_ = lambda a, b_=None: r.t[:, a:(a + 1 if b_ is None else b_)]
                            col = (b * NT + i) * 2
                            ps = PS_C.next()
                            op("pe", lambda t: t.matmul(ps.t[:, 0:32], lhsT=ltri_b.t[:], rhs=ohs.t[:], start=True, stop=True), reads=[ltri_b, ohs], writes=[ps])
                            op("pe", lambda t: t.matmul(ps.t[:, 32:64], lhsT=ones_b.t[:], rhs=ohs.t[:], start=True, stop=True), reads=[ones_b, ohs], writes=[ps])
                            op("dve", lambda v: v.tensor_tensor(out=R_(160, 192), in0=ps.t[:, 0:32], in1=carry.t[:], op=ALU.add), reads=[ps, carry], writes=[r])
                            op("dve", lambda v: v.tensor_tensor(out=carry.t[:], in0=carry.t[:], in1=ps.t[:, 32:64], op=ALU.add), reads=[ps, carry], writes=[carry])
                            for k in range(2):
                                op("dve", lambda v: v.tensor_tensor(out=R_(192, 224), in0=R_(96 + 32 * k, 128 + 32 * k), in1=R_(160, 192), op=ALU.mult), reads=[r], writes=[r])
                                op("dve", lambda v: v.tensor_reduce(out=POSA.t[:, col + k:col + k + 1], in_=R_(192, 224), axis=AX.X, op=ALU.add), reads=[r], writes=[POSA])
                                op("dve", lambda v: v.tensor_tensor(out=R_(192, 224), in0=R_(96 + 32 * k, 128 + 32 * k), in1=ec_t.t[:], op=ALU.mult), reads=[r, ec_t], writes=[r])
                                op("dve", lambda v: v.tensor_reduce(out=EXPA.t[:, col + k:col + k + 1], in_=R_(192, 224), axis=AX.X, op=ALU.add), reads=[r], writes=[EXPA])

                        for i in range(NT + 2):
                            if i < NT:
                                stage1(i)
                            if 1 <= i <= NT:
                                stage2(i - 1)
                            if i >= 2:
                                stage3(i - 2)
                        S.barrier("E")
        if stop_after in ("A", "B", "C", "D"):
            S.finish("sp")
            return nc, ins, dbg_out

        NC2 = NB * NT * 2
        with ExitStack() as em:
            pt_ = sbt(em, "pt_", [128, 8, 32], F32)
            pti = sbt(em, "pti", [128, 32], I32)
            op("dve", lambda v: v.tensor_scalar(out=pt_.t[:, 0, :], in0=carry.t[:], scalar1=float(BS - 1), scalar2=None, op0=ALU.add), reads=[carry], writes=[pt_])
            op("dve", lambda v: v.tensor_copy(out=pti.t[:], in_=pt_.t[:, 0, :]), reads=[pt_], writes=[pti])
            op("dve", lambda v: v.tensor_single_scalar(out=pti.t[:], in_=pti.t[:], scalar=BS_SH, op=ALU.arith_shift_right), reads=[pti], writes=[pti])
            op("dve", lambda v: v.tensor_single_scalar(out=pti.t[:], in_=pti.t[:], scalar=BS_SH, op=ALU.logical_shift_left), reads=[pti], writes=[pti])
            op("dve", lambda v: v.tensor_copy(out=pt_.t[:, 1, :], in_=pti.t[:]), reads=[pti], writes=[pt_])
            op("dve", lambda v: v.memset(pt_.t[:, 4, :], 1.0), reads=[pt_], writes=[pt_])
            op("dve", lambda v: v.tensor_tensor_scan(out=pt_.t[:, 2, :], data0=pt_.t[:, 4, :], data1=pt_.t[:, 1, :], initial=0.0, op0=ALU.mult, op1=ALU.add), reads=[pt_], writes=[pt_])
            op("dve", lambda v: v.tensor_tensor(out=pt_.t[:, 3, :], in0=pt_.t[:, 2, :], in1=pt_.t[:, 1, :], op=ALU.subtract), reads=[pt_], writes=[pt_])
            cmpb = sbt(em, "cmpb", [128, NBLK, 32], F32)
            bexp = sbt(em, "bexp", [128, NBLK], F32)
            widx = sbt(em, "widx", [128, NBLK], I32)
            op("dve", lambda v: v.tensor_tensor(out=cmpb.t[:], in0=pt_.t[:, 2, :].unsqueeze(1).to_broadcast([128, NBLK, 32]), in1=blk0_t.t[:].unsqueeze(2).to_broadcast([128, NBLK, 32]), op=ALU.is_le), reads=[pt_, blk0_t], writes=[cmpb])
            op("dve", lambda v: v.tensor_reduce(out=bexp.t[:], in_=cmpb.t[:], axis=AX.X, op=ALU.add), reads=[cmpb], writes=[bexp])
            op("dve", lambda v: v.tensor_scalar(out=bexp.t[:], in0=bexp.t[:], scalar1=31.0, scalar2=128.0, op0=ALU.min, op1=ALU.mult), reads=[bexp], writes=[bexp])
            op("dve", lambda v: v.tensor_scalar(out=bexp.t[:], in0=bexp.t[:], scalar1=tokid_t.t[:, 0:1], scalar2=None, op0=ALU.add), reads=[bexp, tokid_t], writes=[bexp])
            op("dve", lambda v: v.tensor_copy(out=widx.t[:], in_=bexp.t[:]), reads=[bexp], writes=[widx])
            with ExitStack() as em0:
                oha = sbt(em0, "oha", [128, NC2, 32], F32)
                dst = sbt(em0, "dst", [128, NC2], F32)
                dsti = sbt(em0, "dsti", [128, NC2], I32)
                op("dve", lambda v: v.tensor_tensor(out=oha.t[:], in0=EXPA.t[:].unsqueeze(2).to_broadcast([128, NC2, 32]), in1=ec_t.t[:].unsqueeze(1).to_broadcast([128, NC2, 32]), op=ALU.is_equal), reads=[EXPA, ec_t], writes=[oha])
                op("dve", lambda v: v.tensor_tensor(out=oha.t[:], in0=oha.t[:], in1=pt_.t[:, 3, :].unsqueeze(1).to_broadcast([128, NC2, 32]), op=ALU.mult), reads=[oha, pt_], writes=[oha])
                op("dve", lambda v: v.tensor_reduce(out=dst.t[:], in_=oha.t[:], axis=AX.X, op=ALU.add), reads=[oha], writes=[dst])
                op("dve", lambda v: v.tensor_tensor(out=dst.t[:], in0=dst.t[:], in1=POSA.t[:], op=ALU.add), reads=[dst, POSA], writes=[dst])
                op("dve", lambda v: v.tensor_copy(out=dsti.t[:], in_=dst.t[:]), reads=[dst], writes=[dsti])
                for c_ in range(NC2):
                    S.scatter(SLOT_d.ap()[:, :], REC.t[:, c_, :], dsti.t[:, c_:c_ + 1], reads=[REC, dsti, SLb], writes=[Buf()])
                S.barrier("slots")
            if "moe" in dbg:
                S.dma("sp", ddbg("pt", [128, 8 * 32]).ap()[:, :], pt_.t[:].rearrange("p a b -> p (a b)"), reads=[pt_])
                S.dma("sp", ddbg("widx", [128, NBLK], I32).ap()[:, :], widx.t[:], reads=[widx])
            w1r = Ring([sbt(em, "ew1_%d" % i, [128, 8, 512], BF16) for i in range(3)])
            w3r = Ring([sbt(em, "ew3_%d" % i, [128, 8, 512], BF16) for i in range(3)])
            w2r = Ring([sbt(em, "ew2_%d" % i, [128, 4, D], BF16) for i in range(3)])
            NJ = BS // 128
            recs = Ring([sbt(em, "mrec%d" % i, [128, NJ, 2], F32) for i in range(3)])
            idxs = Ring([sbt(em, "midx%d" % i, [128, NJ], I32) for i in range(3)])
            xg = Ring([sbt(em, "xg%d" % i, [128, D], BF16) for i in range(3 * (BS // 128))])
            xgT = Ring([sbt(em, "xgT%d" % i, [128, 8, BS], BF16) for i in range(2)])
            GT = Ring([sbt(em, "GT%d" % i, [128, 4, BS], BF16) for i in range(2)])
            s1 = Ring([sbt(em, "s1_%d" % i, [128, BS], F32) for i in range(2)])
            yw = Ring([sbt(em, "yw%d" % i, [128, D], F32) for i in range(4)])
            ew1v = EWB_d[0].ap()[:, :]
            ew3v = EWB_d[1].ap()[:, :]
            ew2v = EWB_d[2].ap()[:, :]
            state = {}

            def prepA(blk):
                w1 = w1r.next(); w3 = w3r.next(); w2 = w2r.next()
                S.gather(w1.t[:].rearrange("p k n -> p (k n)"), ew1v, widx.t[:, blk:blk + 1], reads=[widx], writes=[w1])
                S.gather(w3.t[:].rearrange("p k n -> p (k n)"), ew3v, widx.t[:, blk:blk + 1], reads=[widx], writes=[w3])
                S.gather(w2.t[:].rearrange("p k n -> p (k n)"), ew2v, widx.t[:, blk:blk + 1], reads=[widx], writes=[w2])
                rc = recs.next(); ix = idxs.next()
                S.dma("sp", rc.t[:], SLOT_d.ap()[blk * BS:(blk + 1) * BS, :].rearrange("(p j) c -> p j c", p=128), writes=[rc])
                op("dve", lambda v: v.tensor_copy(out=ix.t[:], in_=rc.t[:, :, 0]), reads=[rc], writes=[ix])
                gs = []
                for j in range(NJ):
                    g_ = xg.next()
                    S.gather(g_.t[:], XM_d.ap()[:, :], ix.t[:, j:j + 1], reads=[ix], writes=[g_])
                    gs.append(g_)
                state[blk] = dict(w1=w1, w3=w3, w2=w2, rc=rc, ix=ix, gs=gs)

            def prepB(blk):
                st_ = state[blk]
                xt_ = xgT.next()
                for j in range(NJ):
                    g_ = st_["gs"][j]
                    ps = PS_C.next()
                    psv = ps.t[:].bitcast(BF16)
                    for kc in range(8):
                        op("pe", lambda t: t.transpose(out=psv[:, kc * 128:(kc + 1) * 128], in_=g_.t[:, kc:D:8], identity=ident_b.t[:]), reads=[g_, ident_b], writes=[ps])
                    op("act", lambda a: a.activation(out=xt_.t[:, :, j * 128:(j + 1) * 128], in_=psv.rearrange("p (k t) -> p k t", k=8), func=AF.Copy), reads=[ps], writes=[xt_])
                st_["xt"] = xt_

            def computeH(blk):
                st_ = state[blk]
                w1, w3, xt_ = st_["w1"], st_["w3"], st_["xt"]
                gt = GT.next()
                st_["gt"] = gt
                for fc in range(4):
                    p1 = PS_A.next()
                    for kc in range(8):
                        op("pe", lambda t: t.matmul(p1.t[:, 0:BS], lhsT=w1.t[:, kc, fc:512:4], rhs=xt_.t[:, kc, :], start=(kc == 0), stop=(kc == 7)), reads=[w1, xt_], writes=[p1])
                    p3 = PS_B.next()
                    for kc in range(8):
                        op("pe", lambda t: t.matmul(p3.t[:, 0:BS], lhsT=w3.t[:, kc, fc:512:4], rhs=xt_.t[:, kc, :], start=(kc == 0), stop=(kc == 7)), reads=[w3, xt_], writes=[p3])
                    s_ = s1.next()
                    op("act", lambda a: a.activation(out=s_.t[:], in_=p1.t[:, 0:BS], func=AF.Silu), reads=[p1], writes=[s_])
                    op("dve", lambda v: v.tensor_tensor(out=gt.t[:, fc, :], in0=s_.t[:], in1=p3.t[:, 0:BS], op=ALU.mult), reads=[s_, p3], writes=[gt])

            def computeY(blk):
                st_ = state.pop(blk)
                w2, rc, ix, gt = st_["w2"], st_["rc"], st_["ix"], st_["gt"]
                for ev_ in prev_sc:
                    S._wait("pool", ev_)
                del prev_sc[:]
                for j in range(NJ):
                    y_ = yw.next()
                    for hf in range(2):
                        ps = PS_A.next()
                        for fc in range(4):
                            op("pe", lambda t: t.matmul(ps.t[:], lhsT=gt.t[:, fc, j * 128:(j + 1) * 128], rhs=w2.t[:, fc, hf * 512:(hf + 1) * 512], start=(fc == 0), stop=(fc == 3)), reads=[gt, w2], writes=[ps])
                        if hf == 0:
                            op("act", lambda a: a.activation(out=y_.t[:, hf * 512:(hf + 1) * 512], in_=ps.t[:], func=AF.Copy, scale=rc.t[:, j, 1:2]), reads=[ps, rc], writes=[y_])
                        else:
                            op("dve", lambda v: v.tensor_scalar(out=y_.t[:, hf * 512:(hf + 1) * 512], in0=ps.t[:], scalar1=rc.t[:, j, 1:2], scalar2=None, op0=ALU.mult), reads=[ps, rc], writes=[y_])
                    prev_sc.append(S.scatter(out_d.ap()[:, :], y_.t[:], ix.t[:, j:j + 1], reads=[y_, ix], writes=[Buf()], add=True))

            prev_sc = []
            prepA(0)
            if NBLK > 1:
                prepA(1)
            prepB(0)
            for blk in range(NBLK):
                if blk + 2 < NBLK:
                    prepA(blk + 2)
                computeH(blk)
                if blk + 1 < NBLK:
                    prepB(blk + 1)
                computeY(blk)
            S.finish("sp")
    return nc, ins, dbg_out


def _core_inputs(inp, NB, b0, cst=None):
    f = lambda a: np.ascontiguousarray(np.asarray(a, np.float32))
    m = {}
    m["x"] = f(inp["x"][b0:b0 + NB]).reshape(NB * T, D)
    m["mem"] = f(inp["mem"][b0:b0 + NB]).reshape(NB * ML, D)
    return m


def _shared_inputs(inp, NB):
    f = lambda a: np.ascontiguousarray(np.asarray(a, np.float32))
    m = {}
    m["rel_bias"] = f(inp["rel_bias"])
    m["w_in"] = f(inp["w_in"][0])
    m["vecs"] = _pack_vecs(inp)
    m["gmoe_b"] = np.ascontiguousarray(np.broadcast_to(f(inp["norm_moe"][0])[None], (128, D)))
    brc = np.concatenate([f(inp["router_g_b"][0]), f(inp["router_e_b"][0])])
    m["br_b"] = np.ascontiguousarray(np.broadcast_to(brc[None], (128, 36)))
    m["wbd_r"] = _bd(inp["rg_w_r"][0]); m["wbd_i"] = _bd(inp["rg_w_i"][0])
    m["posT_k"] = f(np.asarray(inp["cmp_pos_k"][0]).T); m["posT_v"] = f(np.asarray(inp["cmp_pos_v"][0]).T)
    m["ck_w1"] = f(inp["cmp_k_w1"][0]); m["ck_w2"] = f(inp["cmp_k_w2"][0])
    m["cv_w1"] = f(inp["cmp_v_w1"][0]); m["cv_w2"] = f(inp["cmp_v_w2"][0])
    m["w_out"] = f(inp["w_out"][0]); m["xa_wq"] = f(inp["xa_w_q"][0]); m["xa_wkv"] = f(inp["xa_w_kv"][0]); m["xa_wo"] = f(inp["xa_w_o"][0])
    m["wr_cat"] = np.ascontiguousarray(np.concatenate([f(inp["router_g_w"][0]), f(inp["router_e_w"][0])], axis=1))
    m["exp_w1"] = f(inp["exp_w1"][0]); m["exp_w3"] = f(inp["exp_w3"][0]); m["exp_w2"] = f(inp["exp_w2"][0])
    m.update(_static_consts(NB))
    return m


def kernel(**inputs):
    NB = 32 // N_CORES
    nc, ins, _ = build(NB)
    shared = _shared_inputs(inputs, NB)
    in_maps = []
    for c in range(N_CORES):
        m = dict(shared)
        m.update(_core_inputs(inputs, NB, c * NB))
        in_maps.append(m)
    res = run_bass_kernel_spmd(nc, in_maps, core_ids=list(range(N_CORES)))
    outs = [np.asarray(r["out"])[:NB * T].reshape(NB, T, D) for r in res.results]
    return np.concatenate(outs, axis=0).astype(np.float32)
```
